# Optimizing a Trainium2 kernel written in Bass

```python
import jax, jax.numpy as jnp
from jax import lax
import numpy as np

D_MODEL = 1024
BATCH = 8
SEQ = 2048
DEPTH = 2

CHUNK = 64
Q_BLOCK = 128
N_MIXERS = 2
EPS = 1e-6
A_HEADS = 8
A_LAT = 128
IDX_HEADS = 8
IDX_DIM = 64
TOPK_MAX = 256
A_Q_COLS = A_HEADS * A_LAT
A_IN_COLS = A_Q_COLS + A_LAT + IDX_HEADS * IDX_DIM + IDX_DIM + IDX_HEADS
B_HEADS = D_MODEL // 256
B_DK = D_MODEL // B_HEADS
B_DV = 2 * B_DK
B_IN_COLS = 2 * D_MODEL + 2 * (B_HEADS * B_DV)
ROPE_BASE = 10000.0
D_FF = 2816
CONV_W = 3
N_A = (DEPTH + N_MIXERS - 1) // N_MIXERS
N_B = DEPTH // N_MIXERS

kernel_name = 'hybrid_dsa_retention_convffn'


def rms_norm(x, g):
    xf = x.astype(jnp.float32)
    y = xf * lax.rsqrt(jnp.mean(xf * xf, axis=-1, keepdims=True) + EPS)
    return (y * g.astype(jnp.float32)).astype(x.dtype)


def dsa_mixer(h, w_in, q_g, k_g, iq_g, ik_g, w_out):
    bsz, seq, _ = h.shape
    topk = min(TOPK_MAX, seq // 4)
    proj = h @ w_in
    o1 = A_Q_COLS
    o2 = o1 + A_LAT
    o3 = o2 + IDX_HEADS * IDX_DIM
    o4 = o3 + IDX_DIM
    q = proj[..., :o1].reshape(bsz, seq, A_HEADS, A_LAT)
    c = proj[..., o1:o2]
    qi = proj[..., o2:o3].reshape(bsz, seq, IDX_HEADS, IDX_DIM)
    ki = rms_norm(proj[..., o3:o4], ik_g)
    wi = proj[..., o4:] * (IDX_HEADS ** -0.5 * IDX_DIM ** -0.5)
    q = rms_norm(q, q_g) * (A_LAT ** -0.5)
    k = rms_norm(c, k_g)
    qi = rms_norm(qi, iq_g)

    nb = seq // Q_BLOCK

    def to_blocks(a):
        return a.reshape(bsz, nb, Q_BLOCK, *a.shape[2:]).swapaxes(0, 1)

    t_blk = jnp.arange(seq, dtype=jnp.int32).reshape(nb, Q_BLOCK)
    key_pos = jnp.arange(seq, dtype=jnp.int32)
    gather = jax.vmap(lambda a, idx: a[idx])

    def block(args):
        qb, qib, wb, tb = args
        limit = (tb // CHUNK + 1) * CHUNK
        admissible = key_pos[None, :] < limit[:, None]
        rel = jax.nn.relu(jnp.einsum('bqhd,bsd->bqhs', qib, ki).astype(jnp.float32))
        score = jnp.einsum('bqhs,bqh->bqs', rel, wb.astype(jnp.float32))
        score = jnp.where(admissible[None], score, -jnp.inf)
        _, idx = lax.top_k(score, topk)
        valid = idx < limit[None, :, None]
        k_sel = gather(k, idx)
        v_sel = gather(c, idx)
        logits = jnp.einsum('bqhd,bqkd->bqhk', qb, k_sel).astype(jnp.float32)
        logits = jnp.where(valid[:, :, None, :], logits, -jnp.inf)
        p = jax.nn.softmax(logits, axis=-1).astype(v_sel.dtype)
        return jnp.einsum('bqhk,bqkd->bqhd', p, v_sel)

    o = lax.map(block, (to_blocks(q), to_blocks(qi), to_blocks(wi), t_blk))
    o = o.swapaxes(0, 1).reshape(bsz, seq, A_Q_COLS)
    return o @ w_out


def rotary(x):
    seq, d = x.shape[1], x.shape[-1]
    inv = 1.0 / (ROPE_BASE ** (jnp.arange(0, d, 2, dtype=jnp.float32) / d))
    ang = jnp.arange(seq, dtype=jnp.float32)[:, None] * inv[None, :]
    cos = jnp.cos(ang)[None, :, None, :]
    sin = jnp.sin(ang)[None, :, None, :]
    xf = x.astype(jnp.float32)
    x1, x2 = xf[..., : d // 2], xf[..., d // 2:]
    return jnp.concatenate([x1 * cos - x2 * sin, x1 * sin + x2 * cos], axis=-1)


def retention_mixer(h, w_in, out_g, w_out):
    bsz, seq, _ = h.shape
    nc = seq // CHUNK
    proj = h @ w_in
    dq = B_HEADS * B_DK
    dv = B_HEADS * B_DV
    q = rotary(proj[..., :dq].reshape(bsz, seq, B_HEADS, B_DK))
    k = rotary(proj[..., dq:2 * dq].reshape(bsz, seq, B_HEADS, B_DK)) * (B_DK ** -0.5)
    v = proj[..., 2 * dq:2 * dq + dv].reshape(bsz, seq, B_HEADS, B_DV).astype(jnp.float32)
    g = proj[..., 2 * dq + dv:]

    log_gamma = jnp.log1p(-(2.0 ** (-5.0 - jnp.arange(B_HEADS, dtype=jnp.float32))))
    pos = jnp.arange(CHUNK, dtype=jnp.float32)
    diff = pos[:, None] - pos[None, :]
    decay_intra = jnp.where(diff[None] >= 0,
                            jnp.exp(jnp.maximum(diff, 0.0)[None] * log_gamma[:, None, None]), 0.0)
    xi = jnp.exp((pos + 1.0)[:, None] * log_gamma[None, :])
    zeta = jnp.exp((CHUNK - 1.0 - pos)[:, None] * log_gamma[None, :])
    gamma_c = jnp.exp(CHUNK * log_gamma)

    qc = q.reshape(bsz, nc, CHUNK, B_HEADS, B_DK)
    kc = k.reshape(bsz, nc, CHUNK, B_HEADS, B_DK)
    vc = v.reshape(bsz, nc, CHUNK, B_HEADS, B_DV)
    s = jnp.einsum('bnqhd,bnkhd->bnhqk', qc, kc) * decay_intra[None, None]
    intra = jnp.einsum('bnhqk,bnkhe->bnqhe', s, vc)

    def step(state, inp):
        qn, kn, vn = inp
        cross = jnp.einsum('bqhd,bhde->bqhe', qn, state) * xi[None, :, :, None]
        state = state * gamma_c[None, :, None, None] + jnp.einsum(
            'bkhd,bkhe->bhde', kn * zeta[None, :, :, None], vn)
        return state, cross

    state0 = jnp.zeros((bsz, B_HEADS, B_DK, B_DV), jnp.float32)
    _, cross = lax.scan(step, state0, (qc.swapaxes(0, 1), kc.swapaxes(0, 1), vc.swapaxes(0, 1)))
    ret = (intra + cross.swapaxes(0, 1)).reshape(bsz, seq, B_HEADS, B_DV)
    ret = rms_norm(ret, out_g.reshape(B_HEADS, B_DV)).reshape(bsz, seq, dv)
    y = ret * jax.nn.silu(g.astype(jnp.float32))
    return y.astype(h.dtype) @ w_out


def conv_ffn(h, w_up, conv_w, conv_b, w_down):
    seq = h.shape[1]
    u = h @ w_up
    up = jnp.pad(u, ((0, 0), (CONV_W - 1, 0), (0, 0)))
    u = sum(up[:, j:j + seq] * conv_w[j] for j in range(CONV_W)) + conv_b
    a, b = u[..., :D_FF], u[..., D_FF:]
    return (jax.nn.silu(a) * b) @ w_down


def setup_inputs(seed: int = 0) -> dict:
    key = jax.random.key(seed)
    ks = jax.random.split(key, 16)

    def nrm(k, shape, scale):
        return jax.random.normal(k, shape, jnp.float32) * scale

    return {
        'x': nrm(ks[0], (BATCH, SEQ, D_MODEL), 1.0),
        'norm_mix_g': 1.0 + nrm(ks[1], (DEPTH, D_MODEL), 0.02),
        'norm_ffn_g': 1.0 + nrm(ks[2], (DEPTH, D_MODEL), 0.02),
        'a_w_in': nrm(ks[3], (N_A, D_MODEL, A_IN_COLS), D_MODEL ** -0.5),
        'a_q_g': 1.0 + nrm(ks[4], (N_A, A_LAT), 0.02),
        'a_k_g': 1.0 + nrm(ks[5], (N_A, A_LAT), 0.02),
        'a_iq_g': 1.0 + nrm(ks[6], (N_A, IDX_DIM), 0.02),
        'a_ik_g': 1.0 + nrm(ks[7], (N_A, IDX_DIM), 0.02),
        'a_w_out': nrm(ks[8], (N_A, A_Q_COLS, D_MODEL), A_Q_COLS ** -0.5),
        'b_w_in': nrm(ks[9], (N_B, D_MODEL, B_IN_COLS), D_MODEL ** -0.5),
        'b_out_g': 1.0 + nrm(ks[10], (N_B, B_HEADS * B_DV), 0.02),
        'b_w_out': nrm(ks[11], (N_B, B_HEADS * B_DV, D_MODEL), (B_HEADS * B_DV) ** -0.5),
        'f_w_up': nrm(ks[12], (DEPTH, D_MODEL, 2 * D_FF), D_MODEL ** -0.5),
        'f_conv_w': nrm(ks[13], (DEPTH, CONV_W, 2 * D_FF), CONV_W ** -0.5),
        'f_conv_b': nrm(ks[14], (DEPTH, 2 * D_FF), 0.01),
        'f_w_down': nrm(ks[15], (DEPTH, D_FF, D_MODEL), D_FF ** -0.5),
    }


def reference(x, norm_mix_g, norm_ffn_g, a_w_in, a_q_g, a_k_g, a_iq_g, a_ik_g, a_w_out,
              b_w_in, b_out_g, b_w_out, f_w_up, f_conv_w, f_conv_b, f_w_down):
    for i in range(DEPTH):
        h = rms_norm(x, norm_mix_g[i])
        j = i // N_MIXERS
        if i % N_MIXERS == 0:
            x = x + dsa_mixer(h, a_w_in[j], a_q_g[j], a_k_g[j], a_iq_g[j], a_ik_g[j], a_w_out[j])
        else:
            x = x + retention_mixer(h, b_w_in[j], b_out_g[j], b_w_out[j])
        x = x + conv_ffn(rms_norm(x, norm_ffn_g[i]), f_w_up[i], f_conv_w[i], f_conv_b[i], f_w_down[i])
    return x
```

```python
from contextlib import ExitStack

import numpy as np
import concourse.bass as bass
import concourse.mybir as mybir
from concourse.bass_utils import run_bass_kernel_spmd

F32 = mybir.dt.float32
BF16 = mybir.dt.bfloat16
AF = mybir.ActivationFunctionType
ALU = mybir.AluOpType
AX = mybir.AxisListType

ENGS = ("pe", "dve", "act", "pool", "sp")
NDMASEM = 8


class T:
    __slots__ = ("ap", "lw", "rd", "name")

    def __init__(self, ap, name=""):
        self.ap = ap
        self.lw = None
        self.rd = {}
        self.name = name

    def __getitem__(self, idx):
        return V(self, self.ap[idx])

    def v(self, ap=None):
        return V(self, self.ap if ap is None else ap)


class V:
    __slots__ = ("ts", "ap")

    def __init__(self, t, ap):
        self.ts = t if isinstance(t, (list, tuple)) else [t]
        self.ap = ap

    def __getitem__(self, idx):
        return V(self.ts, self.ap[idx])

    def re(self, s, **kw):
        return V(self.ts, self.ap.rearrange(s, **kw))

    def bc(self, shape):
        return V(self.ts, self.ap.to_broadcast(shape))


class Op:
    __slots__ = ("eng", "pos", "fn", "deps", "sig", "dma", "dkey", "dval", "vc", "semval", "tag")


class Prog:
    def __init__(self, nc):
        self.nc = nc
        self.ops = {e: [] for e in ENGS}
        self.clk = {e: {} for e in ENGS}
        self.ndma = {e: 0 for e in ENGS}
        self.dma_last = {}

    def _need(self, op, p, clk):
        if p is None or p is op:
            return
        if p.dma:
            key, val = p.dkey, p.dval
        else:
            key, val = p.eng, p.pos
        if clk.get(key, 0) >= val:
            return
        op.deps.append(p)
        p.sig = True
        for k, v in p.vc.items():
            if clk.get(k, 0) < v:
                clk[k] = v

    def add(self, eng, fn, reads=(), writes=(), dma=False):
        op = Op()
        op.eng = eng
        op.fn = fn
        op.tag = ''
        op.deps = []
        op.sig = False
        op.dma = dma
        op.pos = len(self.ops[eng]) + 1
        clk = self.clk[eng]
        for t in reads:
            self._need(op, t.lw, clk)
        for t in writes:
            w = t.lw
            if w is not None and (w.dma or dma or w.eng != eng or eng != "pe"):
                self._need(op, w, clk)
            for k, r in t.rd.items():
                if r.dma or dma or r.eng != eng or eng != "pe":
                    self._need(op, r, clk)
        if dma:
            j = self.ndma[eng]
            self.ndma[eng] = j + 1
            slot = j % NDMASEM
            prev = self.dma_last.get((eng, slot))
            if prev is not None:
                self._need(op, prev, clk)
            op.dkey = ("dma", eng, slot)
            op.dval = 16 * (j // NDMASEM + 1)
            self.dma_last[(eng, slot)] = op
            op.vc = dict(clk)
            op.vc[op.dkey] = op.dval
        else:
            op.vc = dict(clk)
            op.vc[eng] = op.pos
        for t in reads:
            if dma:
                t.rd[("dma", eng, op.pos)] = op
            else:
                t.rd[eng] = op
        for t in writes:
            t.lw = op
            t.rd = {}
        self.ops[eng].append(op)
        return op

    def barrier(self):
        lasts = []
        for e in ENGS:
            for o in reversed(self.ops[e]):
                if not o.dma and o.fn is not None:
                    lasts.append(o)
                    break
        dmas = list(self.dma_last.values())
        for e in ENGS:
            op = Op()
            op.eng = e
            op.fn = None
            op.tag = 'barrier'
            op.deps = []
            op.sig = False
            op.dma = False
            op.pos = len(self.ops[e]) + 1
            clk = self.clk[e]
            for p in lasts + dmas:
                if p.eng == e and not p.dma:
                    continue
                self._need(op, p, clk)
            op.vc = dict(clk)
            op.vc[e] = op.pos
            self.ops[e].append(op)

    _W = ("out", "accum_out", "ap")

    def I(self, eng, method, *args, **kw):
        reads, writes, a = [], [], {}
        for k, v in kw.items():
            if isinstance(v, V):
                (writes if k in self._W else reads).extend(v.ts)
                a[k] = v.ap
            else:
                a[k] = v
        pa = []
        for v in args:
            assert not isinstance(v, V)
            pa.append(v)
        op = self.add(eng, lambda e: getattr(e, method)(*pa, **a), reads, writes)
        op.tag = method + ' ' + ','.join(t.name for t in writes) + ' <- ' + ','.join(t.name for t in reads)
        return op

    def dma(self, eng, out, in_, **kw):
        reads, writes = [], []
        if isinstance(out, V):
            writes.extend(out.ts)
            out = out.ap
        if isinstance(in_, V):
            reads.extend(in_.ts)
            in_ = in_.ap
        return self.add(eng, lambda e: e.dma_start(out=out, in_=in_, **kw), reads, writes, dma=True)

    def emit(self, stack):
        nc = self.nc
        sem = {e: stack.enter_context(nc.semaphore("s_" + e)) for e in ENGS}
        dsem = {}
        for e in ENGS:
            if self.ndma[e]:
                for s in range(min(NDMASEM, self.ndma[e])):
                    dsem[("dma", e, s)] = stack.enter_context(nc.semaphore("d_%s%d" % (e, s)))
        for e in ENGS:
            c = 0
            for o in self.ops[e]:
                if o.sig and not o.dma:
                    c += 1
                o.semval = c
        block = stack.enter_context(nc.Block())

        def run(e, name):
            last = {}
            for o in self.ops[name]:
                for p in o.deps:
                    if p.dma:
                        e.wait_ge(dsem[p.dkey], p.dval)
                    else:
                        e.wait_ge(sem[p.eng], p.semval)
                if o.fn is None:
                    continue
                ins = o.fn(e)
                if o.dma:
                    ins.then_inc(dsem[o.dkey], 16)
                    last[o.dkey] = o.dval
                elif o.sig:
                    ins.then_inc(sem[name], 1)
            for k, v in last.items():
                e.wait_ge(dsem[k], v)

        @block.tensor
        def _(e):
            run(e, "pe")

        @block.vector
        def _(e):
            run(e, "dve")

        @block.scalar
        def _(e):
            run(e, "act")

        @block.gpsimd
        def _(e):
            run(e, "pool")

        @block.sync
        def _(e):
            run(e, "sp")

S = 2048
D = 1024
NT = 16
EPS = 1e-6
DFF = 2816
NPAIR = 22
NIT = 16
DBG = {}
import os as _os, json as _json
if _os.environ.get('KDBG'):
    DBG.update(_json.loads(_os.environ['KDBG']))
ARENA_BYTES = DBG.get('arena_kb', 196) * 1024
FFN_PARTS = [(0, 6), (6, 6), (12, 5), (17, 5)]
CV_MIX = 0
CV_FFN = 2048
CV_QG = 4096
CV_KG = 4224
CV_IQG = 4352
CV_IKG = 4416
CV_OG = 4480
NCV = 6528


def _dsize(dt):
    return 4 if dt == F32 else 2


class Arena:
    def __init__(self, ap_f32, nbytes):
        self.ap = ap_f32
        self.n = nbytes
        self.off = 0

    def alloc_ap(self, shape, dt):
        free = 1
        for s in shape[1:]:
            free *= s
        nb = free * _dsize(dt)
        al = 64 if nb >= 256 else 4
        nb_al = (nb + al - 1) // al * al
        assert self.off + nb_al <= self.n, ("arena overflow", self.off, nb_al, self.n)
        self.hi = max(getattr(self, "hi", 0), self.off + nb_al)
        v = self.ap[:, self.off // 4:(self.off + nb_al) // 4]
        if dt != F32:
            v = v.bitcast(dt)
        v = v[:, 0:free]
        self.off += nb_al
        if len(shape) == 3:
            v = v.rearrange("p (a b) -> p a b", a=shape[1])
        elif len(shape) == 4:
            v = v.rearrange("p (a b c) -> p a b c", a=shape[1], b=shape[2])
        return v

    def tile(self, name, shape, dt):
        return T(self.alloc_ap(shape, dt), name)


NEED = {"dsa": {"a_w_in", "a_w_out"}, "ffn0": {"convw", "f_w_up", "f_w_down"}, "ffn1": {"convw", "f_w_up", "f_w_down"},
        "ret": {"rtab", "dtab", "b_w_in", "b_w_out"}}


def needed_inputs(phases):
    need = {"x", "cvec"}
    for ph in phases:
        need |= NEED[ph]
    return need


def build(phases, load=True, store=True):
    nc = bass.Bass("TRN2", target_bir_lowering=False)
    dr = {}

    need = {"x", "cvec"}
    for ph in phases:
        need |= NEED[ph]

    def din(name, shape):
        if name in need:
            dr[name] = nc.dram_tensor(name, list(shape), F32, kind="ExternalInput").ap()

    din("x", [S, D])
    din("cvec", [128, NCV])
    din("convw", [2, 128, 176])
    din("rtab", [128, 2, NT, 128])
    din("dtab", [128, 8])
    din("a_w_in", [D, 1736])
    din("a_w_out", [D, D])
    din("b_w_in", [D, 4, 1536])
    din("b_w_out", [2048, D])
    din("f_w_up", [2, D, 2 * DFF])
    din("f_w_down", [2, DFF, D])
    out_d = nc.dram_tensor("out", [S, D], F32, kind="ExternalOutput").ap()

    with ExitStack() as es:
        arena_h = es.enter_context(nc.sbuf_tensor("arena", [128, ARENA_BYTES // 4], F32))
        AR = Arena(arena_h.ap(), ARENA_BYTES)
        banks = [T(es.enter_context(nc.psum_tensor("bank%d" % i, [128, 512], F32)).ap(), "bank%d" % i)
                 for i in range(8)]
        P = Prog(nc)

        def bf(bank):
            return V(bank, bank.ap.bitcast(BF16))

        if DBG.get('pad_kb'):
            AR.alloc_ap([128, DBG['pad_kb'] * 256], F32)
        xs_ap = AR.alloc_ap([128, NT, D], F32)
        xs = [T(xs_ap[:, i, :], "x%d" % i) for i in range(NT)]
        ident = AR.tile("ident", [128, 128], BF16)
        ones = AR.tile("ones", [128, 128], BF16)
        cmaskT = AR.tile("cmaskT", [128, 128], BF16)
        P.I("pool", "memset", ap=ident[:], constant=1.0)
        P.I("pool", "affine_select", out=ident[:], in_=ident[:], pattern=[[-1, 128]],
            compare_op=ALU.is_equal, fill=0.0, base=0, channel_multiplier=1)
        P.I("pool", "memset", ap=ones[:], constant=1.0)
        P.I("pool", "memset", ap=cmaskT[:], constant=1.0)
        P.I("pool", "affine_select", out=cmaskT[:], in_=cmaskT[:], pattern=[[1, 128]],
            compare_op=ALU.is_ge, fill=0.0, base=0, channel_multiplier=-1)
        base_mark = AR.off

        x_v = dr["x"].rearrange("(i p) d -> p i d", p=128)
        o_v = out_d.rearrange("(i p) d -> p i d", p=128)
        if load:
            for i in range(NT):
                P.dma("sp", xs[i][:], x_v[:, i, :])

        def norm_T(G, hT_ap, hT_t, tb):
            ss = [AR.tile("ss%d" % i, [128, 1], F32) for i in range(NT)]
            sd = [AR.tile("sd%d" % i, [128, 1], F32) for i in range(NT)]
            rs = [AR.tile("rs%d" % i, [128, 1], F32) for i in range(NT)]
            hb = [AR.tile("hb%d" % k, [128, D], BF16) for k in range(2)]
            for i in range(NT):
                P.I("dve", "memset", ap=ss[i][:], constant=0.0)
                P.I("act", "activation", out=hb[i % 2][:], in_=xs[i][:], func=AF.Square, accum_out=ss[i][:])
                P.I("act", "activation", out=sd[i][:], in_=ss[i][:], func=AF.Sqrt, bias=EPS, scale=1.0 / D)
                P.I("dve", "reciprocal", out=rs[i][:], in_=sd[i][:])
                h = hb[i % 2]
                P.I("dve", "scalar_tensor_tensor", out=h[:], in0=xs[i][:], scalar=rs[i][:], in1=G[:],
                    op0=ALU.mult, op1=ALU.mult)
                bk = banks[tb[i % len(tb)]]
                bkv = bf(bk)
                for c in range(8):
                    P.I("pe", "transpose", out=bkv[:, c * 128:(c + 1) * 128], in_=h[:, c * 128:(c + 1) * 128],
                        identity=ident[:])
                P.I("act", "activation", out=V(hT_t[i], hT_ap[:, :, i * 128:(i + 1) * 128]),
                    in_=bkv.re("p (c t) -> p c t", c=8), func=AF.Copy)

        def alloc_hT():
            hT_ap = AR.alloc_ap([128, 8, S], BF16)
            hT_t = [T(hT_ap[:, :, i * 128:(i + 1) * 128], "hT%d" % i) for i in range(NT)]
            return hT_ap, hT_t

        def phase_ffn(l):
            cw = AR.tile("cw", [128, 4, 44], F32)
            P.dma("sp", cw[:], dr["convw"][l].rearrange("p (a b) -> p a b", a=4))
            hT_ap, hT_t = alloc_hT()
            wu = [AR.tile("wu%d" % k, [128, 8, 2, 6 * 128], BF16) for k in range(2)]
            wd = [AR.tile("wd%d" % k, [128, 6, D], BF16) for k in range(2)]
            ffn_mark = AR.off
            G = AR.tile("G", [128, D], F32)
            P.dma("sp", G[:], dr["cvec"][:, CV_FFN + l * D: CV_FFN + (l + 1) * D])
            wup_v = dr["f_w_up"][l].rearrange("(kc p) n -> p kc n", p=128)
            wdn_v = dr["f_w_down"][l].rearrange("(c p) n -> p c n", p=128)

            def load_part(k):
                p0, npp = FFN_PARTS[k]
                for hf in range(2):
                    for kq in range(4):
                        P.dma("pool", wu[k % 2][:, 2 * kq:2 * kq + 2, hf, 0:npp * 128],
                              wup_v[:, 2 * kq:2 * kq + 2, hf * DFF + p0 * 128: hf * DFF + (p0 + npp) * 128])
                for j0 in range(0, npp, 3):
                    j1 = min(npp, j0 + 3)
                    P.dma("pool", wd[k % 2][:, j0:j1, :], wdn_v[:, p0 + j0:p0 + j1, :])

            load_part(0)
            norm_T(G, hT_ap, hT_t, [6, 7])
            P.barrier()
            AR.off = ffn_mark
            gT = [AR.tile("gT%d" % k, [128, 6, 512], BF16) for k in range(2)]
            va = [AR.tile("va%d" % k, [128, 512], F32) for k in range(2)]
            vb = [AR.tile("vb%d" % k, [128, 512], F32) for k in range(2)]
            halo = [AR.tile("halo%d" % k, [128, 2, 6, 2], F32) for k in range(2)]
            upb = 0
            dnb = 0
            cnt = 0
            for k in range(DBG.get("ffn_parts", len(FFN_PARTS))):
                p0, npp = FFN_PARTS[k]
                if k + 1 < len(FFN_PARTS):
                    load_part(k + 1)
                wuk, wdk = wu[k % 2], wd[k % 2]
                for g in range(4):
                    hTg = V(hT_t[4 * g:4 * g + 4], hT_ap[:, :, g * 512:(g + 1) * 512])
                    gt = gT[cnt % 2]
                    cnt += 1
                    hcur, hprev = halo[g % 2], halo[(g + 1) % 2]
                    for j in range(npp):
                        vv = [va[j % 2], vb[j % 2]]
                        pss = []
                        for hf in range(2):
                            ps = banks[upb % 4]
                            upb += 1
                            pss.append(ps)
                            for kc in range(8):
                                P.I("pe", "matmul", out=ps[:], lhsT=wuk[:, kc, hf, j * 128:(j + 1) * 128],
                                    rhs=hTg[:, kc, :], start=(kc == 0), stop=(kc == 7))
                        SK = DBG.get("skip", ())
                        for hf in range(2):
                            cc = hf * NPAIR + p0 + j
                            P.I("act", "activation", out=vv[hf][:], in_=pss[hf][:], func=AF.Identity,
                                bias=cw[:, 3, cc:cc + 1], scale=cw[:, 2, cc:cc + 1])
                        if "tap" not in SK:
                            for hf in range(2):
                                cc = hf * NPAIR + p0 + j
                                P.I("dve", "scalar_tensor_tensor", out=vv[hf][:, 1:512], in0=pss[hf][:, 0:511],
                                    scalar=cw[:, 1, cc:cc + 1], in1=vv[hf][:, 1:512], op0=ALU.mult, op1=ALU.add)
                            for hf in range(2):
                                cc = hf * NPAIR + p0 + j
                                P.I("dve", "scalar_tensor_tensor", out=vv[hf][:, 2:512], in0=pss[hf][:, 0:510],
                                    scalar=cw[:, 0, cc:cc + 1], in1=vv[hf][:, 2:512], op0=ALU.mult, op1=ALU.add)
                        if "halo" not in SK:
                            if "halo_act" not in SK:
                              for hf in range(2):
                                if DBG.get("halo_on_dve", 1):
                                    P.I("dve", "tensor_copy", out=hcur[:, hf, j, :], in_=pss[hf][:, 510:512])
                                else:
                                    P.I("act", "activation", out=hcur[:, hf, j, :], in_=pss[hf][:, 510:512], func=AF.Copy)
                            if g > 0 and "halo_dve" not in SK:
                                for hf in range(2):
                                    cc = hf * NPAIR + p0 + j
                                    P.I("dve", "scalar_tensor_tensor", out=vv[hf][:, 0:1], in0=hprev[:, hf, j, 1:2],
                                        scalar=cw[:, 1, cc:cc + 1], in1=vv[hf][:, 0:1], op0=ALU.mult, op1=ALU.add)
                                for hf in range(2):
                                    cc = hf * NPAIR + p0 + j
                                    P.I("dve", "scalar_tensor_tensor", out=vv[hf][:, 0:2], in0=hprev[:, hf, j, 0:2],
                                        scalar=cw[:, 0, cc:cc + 1], in1=vv[hf][:, 0:2], op0=ALU.mult, op1=ALU.add)
                        if "silu" not in SK:
                            P.I("act", "activation", out=vv[0][:], in_=vv[0][:], func=AF.Silu)
                        if "gate" not in SK:
                            P.I(DBG.get("gate_eng", "pool"), "tensor_tensor", out=gt[:, j, :], in0=vv[0][:], in1=vv[1][:], op=ALU.mult)
                        else:
                            P.I("dve", "tensor_copy", out=gt[:, j, :], in_=vv[0][:])
                    if "down" in DBG.get("skip", ()):
                        continue
                    for tt in range(4):
                        xi = xs[4 * g + tt]
                        for dh in range(2):
                            ps = banks[4 + dnb % 4]
                            dnb += 1
                            for j in range(npp):
                                P.I("pe", "matmul", out=ps[:], lhsT=gt[:, j, tt * 128:(tt + 1) * 128],
                                    rhs=wdk[:, j, dh * 512:(dh + 1) * 512], start=(j == 0), stop=(j == npp - 1))
                            P.I("dve", "tensor_tensor", out=xi[:, dh * 512:(dh + 1) * 512],
                                in0=xi[:, dh * 512:(dh + 1) * 512], in1=ps[:], op=ALU.add)

        def phase_dsa():
            cv = dr["cvec"]
            gq = AR.tile("gq", [128, 128], F32)
            gk = AR.tile("gk", [128, 128], F32)
            giq = AR.tile("giq", [128, 64], F32)
            gik = AR.tile("gik", [128, 64], F32)
            P.dma("sp", gq[:], cv[:, CV_QG:CV_QG + 128])
            P.dma("sp", gk[:], cv[:, CV_KG:CV_KG + 128])
            P.dma("sp", giq[:], cv[:, CV_IQG:CV_IQG + 64])
            P.dma("sp", gik[:], cv[:, CV_IKG:CV_IKG + 64])
            qT_ap = AR.alloc_ap([128, 8, S], BF16)
            qT = [T(qT_ap[:, :, i * 128:(i + 1) * 128], "qT%d" % i) for i in range(NT)]
            qiT_ap = AR.alloc_ap([128, 4, S], BF16)
            qiT = [T(qiT_ap[:, :, i * 128:(i + 1) * 128], "qiT%d" % i) for i in range(NT)]
            kT_ap = AR.alloc_ap([128, S], BF16)
            kT = [T(kT_ap[:, i * 128:(i + 1) * 128], "kT%d" % i) for i in range(NT)]
            kiT_ap = AR.alloc_ap([128, S], BF16)
            kiT = [T(kiT_ap[:, i * 128:(i + 1) * 128], "kiT%d" % i) for i in range(NT)]
            c_ap = AR.alloc_ap([128, NT, 128], BF16)
            c_t = [T(c_ap[:, i, :], "c%d" % i) for i in range(NT)]
            wi_ap = AR.alloc_ap([128, NT, 8], F32)
            wi_t = [T(wi_ap[:, i, :], "wi%d" % i) for i in range(NT)]
            nb = AR.tile("nbias", [128, 1], F32)
            pw2 = AR.tile("pw2", [128, NIT + 1], F32)
            for it in range(NIT + 1):
                P.I("pool", "memset", ap=pw2[:, it:it + 1], constant=2.0 ** -(it + 1))
            t1 = AR.tile("t1", [128, 128], F32)
            m1 = AR.tile("m1", [128, 1], F32)
            m2 = AR.tile("m2", [128, 1], F32)
            P.I("dve", "tensor_tensor", out=t1[:], in0=gq[:], in1=gq[:], op=ALU.mult)
            P.I("dve", "tensor_reduce", out=m1[:], in_=t1[:], axis=AX.X, op=ALU.max)
            P.I("dve", "tensor_tensor", out=t1[:], in0=gk[:], in1=gk[:], op=ALU.mult)
            P.I("dve", "tensor_reduce", out=m2[:], in_=t1[:], axis=AX.X, op=ALU.max)
            P.I("dve", "tensor_tensor", out=m1[:], in0=m1[:], in1=m2[:], op=ALU.mult)
            P.I("act", "activation", out=m2[:], in_=m1[:], func=AF.Sqrt, scale=128.0)
            P.I("dve", "tensor_scalar", out=nb[:], in0=m2[:], scalar1=-1.0, scalar2=None, op0=ALU.mult)
            mark1 = AR.off

            hT_ap, hT_t = alloc_hT()
            w_in = AR.tile("w_in", [128, 8, 1736], BF16)
            wv = dr["a_w_in"].rearrange("(kc p) n -> p kc n", p=128)
            for kq in range(4):
                P.dma("pool", w_in[:, 2 * kq:2 * kq + 2, :], wv[:, 2 * kq:2 * kq + 2, :])
            mark_n = AR.off
            G = AR.tile("G", [128, D], F32)
            P.dma("sp", G[:], cv[:, CV_MIX:CV_MIX + D])
            norm_T(G, hT_ap, hT_t, [6, 7])
            P.barrier()
            AR.off = mark_n
            ssq = [AR.tile("ssq%d" % k, [128, 18], F32) for k in range(2)]
            sdq = [AR.tile("sdq%d" % k, [128, 18], F32) for k in range(2)]
            rsq = [AR.tile("rsq%d" % k, [128, 18], F32) for k in range(2)]
            sjunk = AR.tile("sjunk", [128, 128], BF16)
            qn = [AR.tile("qn%d" % k, [128, 1024], BF16) for k in range(2)]
            qin = [AR.tile("qin%d" % k, [128, 512], BF16) for k in range(2)]
            kn = [AR.tile("kn%d" % k, [128, 128], BF16) for k in range(2)]
            kin = [AR.tile("kin%d" % k, [128, 128], BF16) for k in range(2)]
            colb = [(0, 512), (512, 1024), (1024, 1536), (1536, 1736)]
            S1 = DBG.get('s1_sub', 'msncte')
            for i in range(DBG.get('s1_tiles', NT)):
                for bi, (c0, c1) in enumerate(colb):
                    for kc in range(8):
                        P.I("pe", "matmul", out=banks[bi][:, 0:c1 - c0], lhsT=hT_t[i].v(hT_ap[:, kc, i * 128:(i + 1) * 128]),
                            rhs=w_in[:, kc, c0:c1], start=(kc == 0), stop=(kc == 7))
                if 's' not in S1:
                    continue
                sq, sdd, rr = ssq[i % 2], sdq[i % 2], rsq[i % 2]
                P.I("dve", "memset", ap=sq[:], constant=0.0)
                for h in range(8):
                    P.I("act", "activation", out=sjunk[:, 0:128], in_=banks[h // 4][:, (h % 4) * 128:(h % 4 + 1) * 128],
                        func=AF.Square, accum_out=sq[:, h:h + 1])
                P.I("act", "activation", out=sjunk[:, 0:128], in_=banks[3][:, 0:128], func=AF.Square,
                    accum_out=sq[:, 8:9])
                for h in range(8):
                    P.I("act", "activation", out=sjunk[:, 0:64], in_=banks[2][:, h * 64:(h + 1) * 64],
                        func=AF.Square, accum_out=sq[:, 9 + h:10 + h])
                P.I("act", "activation", out=sjunk[:, 0:64], in_=banks[3][:, 128:192], func=AF.Square,
                    accum_out=sq[:, 17:18])
                P.I("act", "activation", out=sdd[:, 0:9], in_=sq[:, 0:9], func=AF.Sqrt, bias=EPS, scale=1.0 / 128)
                P.I("act", "activation", out=sdd[:, 9:18], in_=sq[:, 9:18], func=AF.Sqrt, bias=EPS, scale=1.0 / 64)
                P.I("dve", "reciprocal", out=rr[:], in_=sdd[:])
                if 'n' not in S1:
                    continue
                qn_, qin_, kn_, kin_ = qn[i % 2], qin[i % 2], kn[i % 2], kin[i % 2]
                for h in range(8):
                    P.I("dve", "scalar_tensor_tensor", out=qn_[:, h * 128:(h + 1) * 128],
                        in0=banks[h // 4][:, (h % 4) * 128:(h % 4 + 1) * 128], scalar=rr[:, h:h + 1], in1=gq[:],
                        op0=ALU.mult, op1=ALU.mult)
                for h in range(8):
                    P.I("dve", "scalar_tensor_tensor", out=qin_[:, h * 64:(h + 1) * 64],
                        in0=banks[2][:, h * 64:(h + 1) * 64], scalar=rr[:, 9 + h:10 + h], in1=giq[:],
                        op0=ALU.mult, op1=ALU.mult)
                P.I("dve", "scalar_tensor_tensor", out=kn_[:], in0=banks[3][:, 0:128], scalar=rr[:, 8:9], in1=gk[:],
                    op0=ALU.mult, op1=ALU.mult)
                for r in range(2):
                    P.I("dve", "scalar_tensor_tensor", out=kin_[:, r * 64:(r + 1) * 64], in0=banks[3][:, 128:192],
                        scalar=rr[:, 17:18], in1=gik[:], op0=ALU.mult, op1=ALU.mult)
                if 'c' not in S1:
                    continue
                P.I("act", "activation", out=c_t[i][:], in_=banks[3][:, 0:128], func=AF.Copy)
                P.I("act", "activation", out=wi_t[i][:], in_=banks[3][:, 192:200], func=AF.Identity,
                    scale=float(8 ** -0.5 * 64 ** -0.5))
                if 't' not in S1:
                    continue
                tb0 = bf(banks[4 + 2 * (i % 2)])
                tb1 = bf(banks[5 + 2 * (i % 2)])
                for h in range(8):
                    P.I("pe", "transpose", out=tb0[:, h * 128:(h + 1) * 128], in_=qn_[:, h * 128:(h + 1) * 128],
                        identity=ident[:])
                for r in range(4):
                    P.I("pe", "transpose", out=tb1[:, r * 128:(r + 1) * 128], in_=qin_[:, r * 128:(r + 1) * 128],
                        identity=ident[:])
                P.I("pe", "transpose", out=tb1[:, 512:640], in_=kn_[:], identity=ident[:])
                P.I("pe", "transpose", out=tb1[:, 640:768], in_=kin_[:], identity=ident[:])
                if 'e' not in S1:
                    continue
                P.I("act", "activation", out=qT[i][:], in_=tb0.re("p (c t) -> p c t", c=8), func=AF.Copy)
                P.I("act", "activation", out=qiT[i][:], in_=tb1[:, 0:512].re("p (c t) -> p c t", c=4), func=AF.Copy)
                P.I("act", "activation", out=kT[i][:], in_=tb1[:, 512:640], func=AF.Copy)
                P.I("act", "activation", out=kiT[i][:], in_=tb1[:, 640:768], func=AF.Copy)
            P.barrier()
            AR.off = mark1
            if DBG.get("dsa_stage", 9) < 2:
                return

            w_out = AR.tile("w_out", [128, 8, D], BF16)
            wov = dr["a_w_out"].rearrange("(h p) n -> p h n", p=128)
            for kq in range(4):
                P.dma("pool", w_out[:, 2 * kq:2 * kq + 2, :], wov[:, 2 * kq:2 * kq + 2, :])
            score = [AR.tile("score%d" % k, [128, S], F32) for k in range(2)]
            rel = [AR.tile("rel%d" % k, [128, 512], F32) for k in range(2)]
            cjunk = AR.tile("cjunk", [128, S], BF16)
            maskq = AR.tile("maskq", [128, S], BF16)
            maskT = AR.tile("maskT", [128, NT, 128], BF16)
            PT = [AR.tile("PT%d" % k, [128, 512], BF16) for k in range(3)]
            oT = [AR.tile("oT%d" % k, [128, 8, 128], BF16) for k in range(2)]
            rden = [AR.tile("rden%d" % k, [128, 512], F32) for k in range(2)]
            sm = [[AR.tile("sm%d_%d" % (k, q), [128, 1], F32) for q in range(6)] for k in range(2)]
            wtab = [AR.tile("wtab%d" % k, [128, NIT + 1], F32) for k in range(2)]
            maskT2 = [maskT, AR.tile("maskTb", [128, NT, 128], BF16)]
            lnd = [AR.tile("lnd%d" % k, [128, 512], F32) for k in range(2)]
            cnts = {"ib": 0, "lb": 0, "pb": 0}

            def gen_A(b):
                nk = (b + 1) * 128
                sc = score[b % 2]
                mx, mn, w0, thr, cn, dd = sm[b % 2]
                wt = wtab[b % 2]
                mT = maskT2[b % 2]
                nch = (nk + 511) // 512
                for h in range(8):
                    r0 = (h % 2) * 64
                    for kc in range(nch):
                        c0, c1 = kc * 512, min(nk, (kc + 1) * 512)
                        ps = banks[cnts["ib"] % 2]
                        rl = rel[cnts["ib"] % 2]
                        cnts["ib"] += 1
                        kts = kiT[c0 // 128:c1 // 128]
                        P.I("pe", "matmul", out=ps[:, 0:c1 - c0], lhsT=qiT[b].v(qiT_ap[r0:r0 + 64, h // 2, b * 128:(b + 1) * 128]),
                            rhs=V(kts, kiT_ap[r0:r0 + 64, c0:c1]), start=True, stop=True)
                        P.I("act", "activation", out=rl[:, 0:c1 - c0], in_=ps[:, 0:c1 - c0], func=AF.Relu)
                        IE = DBG.get("idx_eng", "pool")
                        if h == 0:
                            P.I(IE, "tensor_scalar", out=sc[:, c0:c1], in0=rl[:, 0:c1 - c0],
                                scalar1=wi_t[b].v(wi_ap[:, b, 0:1]), scalar2=None, op0=ALU.mult)
                        elif IE == "dve":
                            P.I(IE, "scalar_tensor_tensor", out=sc[:, c0:c1], in0=rl[:, 0:c1 - c0],
                                scalar=wi_t[b].v(wi_ap[:, b, h:h + 1]), in1=sc[:, c0:c1], op0=ALU.mult, op1=ALU.add)
                        else:
                            P.I(IE, "tensor_scalar", out=rl[:, 0:c1 - c0], in0=rl[:, 0:c1 - c0],
                                scalar1=wi_t[b].v(wi_ap[:, b, h:h + 1]), scalar2=None, op0=ALU.mult)
                            P.I(IE, "tensor_tensor", out=sc[:, c0:c1], in0=sc[:, c0:c1], in1=rl[:, 0:c1 - c0], op=ALU.add)
                        yield
                if b >= 2:
                    P.I("dve", "tensor_reduce", out=mx[:], in_=sc[:, 0:nk], axis=AX.X, op=ALU.max)
                    P.I("dve", "tensor_reduce", out=mn[:], in_=sc[:, 0:nk], axis=AX.X, op=ALU.min)
                P.I("dve", "memset", ap=sc[0:64, nk - 64:nk], constant=-1e30)
                yield
                if b >= 2:
                    P.I("dve", "tensor_scalar", out=w0[:], in0=mx[:], scalar1=mn[:], scalar2=1.001,
                        op0=ALU.subtract, op1=ALU.mult)
                    P.I("dve", "tensor_scalar", out=wt[:], in0=pw2[:], scalar1=w0[:], scalar2=None, op0=ALU.mult)
                    P.I("dve", "tensor_tensor", out=thr[:], in0=mn[:], in1=wt[:, 0:1], op=ALU.add)
                    for it in range(NIT):
                        P.I("dve", "memset", ap=cn[:], constant=0.0)
                        P.I("dve", "tensor_scalar", out=cjunk[:, 0:nk], in0=sc[:, 0:nk], scalar1=thr[:], scalar2=0.0,
                            op0=ALU.is_ge, op1=ALU.add, accum_out=cn[:])
                        yield
                        P.I("dve", "tensor_scalar", out=dd[:], in0=cn[:], scalar1=255.5, scalar2=0.5,
                            op0=ALU.is_ge, op1=ALU.subtract)
                        P.I("dve", "scalar_tensor_tensor", out=thr[:], in0=dd[:], scalar=wt[:, it:it + 1], in1=thr[:],
                            op0=ALU.mult, op1=ALU.add)
                        yield
                    P.I("dve", "tensor_tensor", out=thr[:], in0=thr[:], in1=wt[:, NIT:NIT + 1], op=ALU.subtract)
                else:
                    P.I("dve", "memset", ap=thr[:], constant=-1e29)
                P.I("dve", "tensor_scalar", out=maskq[:, 0:nk], in0=sc[:, 0:nk], scalar1=thr[:], scalar2=None,
                    op0=ALU.is_ge)
                yield
                bm = bf(banks[4])
                for j0 in range(0, b + 1, 8):
                    j1 = min(b + 1, j0 + 8)
                    for j in range(j0, j1):
                        P.I("pe", "transpose", out=bm[:, (j - j0) * 128:(j - j0 + 1) * 128],
                            in_=maskq[:, j * 128:(j + 1) * 128], identity=ident[:])
                    P.I("act", "activation", out=mT[:, j0:j1, :],
                        in_=bm[:, 0:(j1 - j0) * 128].re("p (c t) -> p c t", c=j1 - j0), func=AF.Copy)
                    yield

            def gen_B(b):
                mT = maskT2[b % 2]
                o_ = oT[b % 2]
                for hh in range(2):
                    rd = rden[hh]
                    for j in range(b + 1):
                        pl = banks[2 + cnts["lb"] % 2]
                        cnts["lb"] += 1
                        pt = PT[cnts["pb"] % 3]
                        cnts["pb"] += 1
                        P.I("pe", "matmul", out=pl[:], lhsT=kT[j][:], rhs=qT[b].v(qT_ap[:, hh * 4:(hh + 1) * 4, b * 128:(b + 1) * 128]),
                            start=True, stop=True)
                        P.I("act", "activation", out=pt[:], in_=pl[:], func=AF.Exp, bias=nb[:], scale=float(128 ** -0.5))
                        P.I("dve", "tensor_tensor", out=pt[:].re("p (h q) -> p h q", h=4),
                            in0=pt[:].re("p (h q) -> p h q", h=4),
                            in1=V(mT, mT.ap[:, j, :].unsqueeze(1).to_broadcast([128, 4, 128])), op=ALU.mult)
                        P.I("pe", "matmul", out=banks[5][:], lhsT=c_t[j][:], rhs=pt[:], start=(j == 0), stop=(j == b))
                        P.I("pe", "matmul", out=banks[6][:], lhsT=ones[:], rhs=pt[:], start=(j == 0), stop=(j == b))
                        yield
                    P.I("act", "activation", out=lnd[hh][:], in_=banks[6][:], func=AF.Ln)
                    P.I("act", "activation", out=rd[:], in_=lnd[hh][:], func=AF.Exp, scale=-1.0)
                    P.I("dve", "tensor_tensor", out=o_[:, hh * 4:(hh + 1) * 4, :].re("p h q -> p (h q)"),
                        in0=banks[5][:], in1=rd[:], op=ALU.mult)
                    yield
                for dh in range(2):
                    for h in range(8):
                        P.I("pe", "matmul", out=banks[7][:], lhsT=o_[:, h, :], rhs=w_out[:, h, dh * 512:(dh + 1) * 512],
                            start=(h == 0), stop=(h == 7))
                    P.I("dve", "tensor_tensor", out=xs[b][:, dh * 512:(dh + 1) * 512],
                        in0=xs[b][:, dh * 512:(dh + 1) * 512], in1=banks[7][:], op=ALU.add)
                    yield

            def _drain(gs):
                gs = list(gs)
                while gs:
                    for g in list(gs):
                        try:
                            next(g)
                        except StopIteration:
                            gs.remove(g)

            NB = DBG.get('dsa_blocks', NT)
            _drain([gen_A(0)])
            for b in range(NB):
                gs = [gen_B(b)]
                if b + 1 < NB:
                    gs.append(gen_A(b + 1))
                _drain(gs)

        def phase_ret():
            cv = dr["cvec"]
            ogh = [AR.tile("ogh%d" % k, [128, 512], F32) for k in range(2)]
            rt = AR.tile("rtab", [128, 2, NT, 128], F32)
            P.dma("sp", rt[:, 0], dr["rtab"][:, 0])
            P.dma("sp", rt[:, 1], dr["rtab"][:, 1])
            dt_ = AR.tile("dtab", [128, 8], F32)
            P.dma("sp", dt_[:], dr["dtab"])
            hT_ap, hT_t = alloc_hT()
            w_in = [AR.tile("rw_in%d" % c, [128, 8, 512], BF16) for c in range(3)]
            w_out = [AR.tile("rw_out%d" % k, [128, 4, D], BF16) for k in range(2)]
            mark_n = AR.off
            G = AR.tile("G", [128, D], F32)
            P.dma("sp", G[:], cv[:, CV_MIX + D:CV_MIX + 2 * D])
            wiv = dr["b_w_in"].rearrange("(kc p) h n -> p kc h n", p=128)
            wov = dr["b_w_out"].rearrange("(h c p) n -> p h c n", p=128, c=4)

            def load_head(h):
                for c in range(3):
                    for kq in range(2):
                        P.dma("pool", w_in[c][:, 4 * kq:4 * kq + 4, :], wiv[:, 4 * kq:4 * kq + 4, h, c * 512:(c + 1) * 512])
                P.dma("pool", w_out[h % 2][:], wov[:, h])
                P.dma("sp", ogh[h % 2][:], cv[:, CV_OG + h * 512:CV_OG + (h + 1) * 512])

            load_head(0)
            norm_T(G, hT_ap, hT_t, [6, 7])
            P.barrier()
            AR.off = mark_n
            U = AR.tile("U", [128, 2, 512], F32)
            sbf = [AR.tile("sbf%d" % k, [128, 2, 512], BF16) for k in range(2)]
            qk = [AR.tile("qk%d" % k, [128, 512], F32) for k in range(2)]
            ra = [AR.tile("ra%d" % k, [128, 2, 128], F32) for k in range(1)] * 2
            rb = [AR.tile("rb%d" % k, [128, 2, 128], F32) for k in range(1)] * 2
            rc = [AR.tile("rc%d" % k, [128, 2, 128], F32) for k in range(1)] * 2
            rd_ = [AR.tile("rd%d" % k, [128, 2, 128], F32) for k in range(1)] * 2
            qkr = [AR.tile("qkr%d" % k, [128, 2, 2, 128], BF16) for k in range(2)]
            qkT = [AR.tile("qkT%d" % k, [128, 4, 128], BF16) for k in range(2)]
            vbf = [AR.tile("vbf%d" % k, [128, 512], BF16) for k in range(2)]
            ST = [AR.tile("ST%d" % k, [128, 128], BF16) for k in range(2)]
            sg = [AR.tile("sg%d" % k, [128, 512], F32) for k in range(2)]
            sg2 = sg
            yb = [AR.tile("yb%d" % k, [128, 512], BF16) for k in range(2)]
            yT = [AR.tile("yT%d" % k, [128, 4, 128], BF16) for k in range(2)]
            rsm = [[AR.tile("rsm%d_%d" % (k, q), [128, 1], F32) for q in range(3)] for k in range(2)]
            rjunk = AR.tile("rjunk", [128, 512], BF16)
            def ret_gen(h, i, k2, g128, wo):
                if i == 0 and h > 0:
                    load_head(h)
                for c in range(3):
                    for kc in range(8):
                        P.I("pe", "matmul", out=banks[c][:], lhsT=hT_t[i].v(hT_ap[:, kc, i * 128:(i + 1) * 128]),
                            rhs=w_in[c][:, kc, :], start=(kc == 0), stop=(kc == 7))
                q_ = qk[k2]
                P.I("act", "activation", out=q_[:, 0:256], in_=banks[0][:, 0:256], func=AF.Identity, scale=dt_[:, h:h + 1])
                P.I("act", "activation", out=q_[:, 256:512], in_=banks[0][:, 256:512], func=AF.Identity,
                    scale=dt_[:, 4 + h:5 + h])
                P.I("act", "activation", out=vbf[k2][:], in_=banks[1][:], func=AF.Copy)
                P.I("act", "activation", out=sg[k2][:], in_=banks[2][:], func=AF.Silu)
                P.I("dve", "tensor_tensor", out=sg2[k2][:], in0=sg[k2][:], in1=ogh[h % 2][:], op=ALU.mult)
                yield None
                qv = q_[:].re("p (a b c) -> p a b c", a=2, b=2)
                X1, X2 = qv[:, :, 0, :], qv[:, :, 1, :]
                cosb = V(rt, rt.ap[:, 0, i, :].unsqueeze(1).to_broadcast([128, 2, 128]))
                sinb = V(rt, rt.ap[:, 1, i, :].unsqueeze(1).to_broadcast([128, 2, 128]))
                P.I("dve", "tensor_tensor", out=ra[k2][:], in0=X1, in1=cosb, op=ALU.mult)
                P.I("dve", "tensor_tensor", out=rb[k2][:], in0=X2, in1=sinb, op=ALU.mult)
                P.I("dve", "tensor_tensor", out=rc[k2][:], in0=X1, in1=sinb, op=ALU.mult)
                P.I("dve", "tensor_tensor", out=rd_[k2][:], in0=X2, in1=cosb, op=ALU.mult)
                P.I("dve", "tensor_tensor", out=qkr[k2][:, :, 0, :], in0=ra[k2][:], in1=rb[k2][:], op=ALU.subtract)
                P.I("dve", "tensor_tensor", out=qkr[k2][:, :, 1, :], in0=rc[k2][:], in1=rd_[k2][:], op=ALU.add)
                yield None
                b3 = bf(banks[3])
                for a in range(2):
                    for c in range(2):
                        P.I("pe", "transpose", out=b3[:, (2 * a + c) * 128:(2 * a + c + 1) * 128],
                            in_=qkr[k2][:, a, c, :], identity=ident[:])
                P.I("act", "activation", out=qkT[k2][:], in_=b3[:, 0:512].re("p (c t) -> p c t", c=4), func=AF.Copy)
                yield None
                at = banks[3][:, 256:384]
                for c in range(2):
                    P.I("pe", "matmul", out=at, lhsT=qkT[k2][:, 2 + c, :], rhs=qkT[k2][:, c, :],
                        start=(c == 0), stop=(c == 1))
                P.I("dve", "tensor_tensor", out=ST[k2][:], in0=at, in1=cmaskT[:], op=ALU.mult)
                yield 'MID'
                P.I("pe", "matmul", out=banks[4][:], lhsT=ST[k2][:], rhs=vbf[k2][:], start=True, stop=(i == 0))
                if i > 0:
                    sb_ = sbf[(i - 1) % 2]
                    for c in range(2):
                        P.I("pe", "matmul", out=banks[4][:], lhsT=qkT[k2][:, c, :], rhs=sb_[:, c, :],
                            start=False, stop=(c == 1))
                yield None
                if i < NT - 1:
                    for c in range(2):
                        P.I("pe", "matmul", out=banks[5 + c][:], lhsT=qkr[k2][:, 1, c, :], rhs=vbf[k2][:],
                            start=True, stop=True)
                    for c in range(2):
                        if i == 0:
                            P.I("dve", "tensor_copy", out=U[:, c, :], in_=banks[5 + c][:])
                        else:
                            P.I("dve", "scalar_tensor_tensor", out=U[:, c, :], in0=U[:, c, :], scalar=g128,
                                in1=banks[5 + c][:], op0=ALU.mult, op1=ALU.add)
                    P.I("act", "activation", out=sbf[i % 2][:], in_=U[:], func=AF.Identity, scale=g128)
                yield None
                ssr, sdr, rsr = rsm[k2]
                P.I("dve", "memset", ap=ssr[:], constant=0.0)
                P.I("act", "activation", out=rjunk[:], in_=banks[4][:], func=AF.Square, accum_out=ssr[:])
                P.I("act", "activation", out=sdr[:], in_=ssr[:], func=AF.Sqrt, bias=EPS, scale=1.0 / 512)
                P.I("dve", "reciprocal", out=rsr[:], in_=sdr[:])
                P.I("dve", "scalar_tensor_tensor", out=yb[k2][:], in0=banks[4][:], scalar=rsr[:], in1=sg2[k2][:],
                    op0=ALU.mult, op1=ALU.mult)
                yield None
                b7 = bf(banks[7])
                for e in range(4):
                    P.I("pe", "transpose", out=b7[:, e * 128:(e + 1) * 128], in_=yb[k2][:, e * 128:(e + 1) * 128],
                        identity=ident[:])
                P.I("act", "activation", out=yT[k2][:], in_=b7[:, 0:512].re("p (c t) -> p c t", c=4), func=AF.Copy)
                for dh in range(2):
                    pso = banks[5 + dh]
                    for e in range(4):
                        P.I("pe", "matmul", out=pso[:], lhsT=yT[k2][:, e, :], rhs=wo[:, e, dh * 512:(dh + 1) * 512],
                            start=(e == 0), stop=(e == 3))
                    P.I("dve", "tensor_tensor", out=xs[i][:, dh * 512:(dh + 1) * 512],
                        in0=xs[i][:, dh * 512:(dh + 1) * 512], in1=pso[:], op=ALU.add)

            def _step(g):
                try:
                    return next(g), True
                except StopIteration:
                    return None, False

            gens = []
            it = 0
            for h in range(4):
                g128 = float((1.0 - 2.0 ** (-5.0 - h)) ** 128)
                for i in range(NT):
                    gens.append(ret_gen(h, i, it % 2, g128, w_out[h % 2]))
                    it += 1
            prev = None
            for g in gens:
                while True:
                    r, alive = _step(g)
                    if prev is not None:
                        _, pa = _step(prev)
                        if not pa:
                            prev = None
                    if r == 'MID' or not alive:
                        break
                while prev is not None:
                    _, pa = _step(prev)
                    if not pa:
                        prev = None
                prev = g
            while prev is not None:
                _, pa = _step(prev)
                if not pa:
                    prev = None

        for ph in phases:
            AR.off = base_mark
            AR.hi = 0
            if ph == "dsa":
                phase_dsa()
            elif ph == "ffn0":
                phase_ffn(0)
            elif ph == "ret":
                phase_ret()
            elif ph == "ffn1":
                phase_ffn(1)
            P.barrier()
            if DBG.get("verbose"):
                print("phase", ph, "arena hi", AR.hi, {e: len(P.ops[e]) for e in ENGS})
        if store:
            for i in range(NT):
                P.dma("sp", o_v[:, i, :], xs[i][:])
        P.emit(es)
    return nc


def _consts():
    d = 256
    inv = (1.0 / (np.float32(10000.0) ** (np.arange(0, d, 2, dtype=np.float32) / np.float32(d)))).astype(np.float32)
    ang = (np.arange(S, dtype=np.float32)[:, None] * inv[None, :]).astype(np.float32)
    cos = np.cos(ang).astype(np.float32)
    sin = np.sin(ang).astype(np.float32)
    rtab = np.stack([cos, sin], 0).reshape(2, NT, 128, 128).transpose(2, 0, 1, 3)
    hh = np.arange(4, dtype=np.float64)
    gam = 1.0 - 2.0 ** (-5.0 - hh)
    tl = np.arange(128, dtype=np.float64)[:, None] + 1.0
    dq = gam[None, :] ** tl
    dk = gam[None, :] ** (-tl) * (256.0 ** -0.5)
    dtab = np.concatenate([dq, dk], 1).astype(np.float32)
    return np.ascontiguousarray(rtab), np.ascontiguousarray(dtab)


def _prep_shared(inp):
    f = lambda a: np.ascontiguousarray(np.asarray(a, dtype=np.float32))
    cvec1 = np.concatenate([
        f(inp["norm_mix_g"]).reshape(-1), f(inp["norm_ffn_g"]).reshape(-1),
        f(inp["a_q_g"]).reshape(-1), f(inp["a_k_g"]).reshape(-1),
        f(inp["a_iq_g"]).reshape(-1), f(inp["a_ik_g"]).reshape(-1),
        f(inp["b_out_g"]).reshape(-1)])
    assert cvec1.size == NCV
    cvec = np.ascontiguousarray(np.broadcast_to(cvec1[None, :], (128, NCV)))
    cw = f(inp["f_conv_w"])
    cb = f(inp["f_conv_b"])
    conv = np.concatenate([cw, cb[:, None, :]], 1)
    convw = np.ascontiguousarray(conv.reshape(2, 4, 44, 128).transpose(0, 3, 1, 2).reshape(2, 128, 176))
    awi = f(inp["a_w_in"])[0]
    a_w_in = np.ascontiguousarray(np.concatenate(
        [awi[:, 0:1024], awi[:, 1152:1664], awi[:, 1024:1152], awi[:, 1664:1728], awi[:, 1728:1736]], 1))
    bwi = f(inp["b_w_in"])[0]
    q = bwi[:, 0:1024].reshape(D, 4, 256)
    k = bwi[:, 1024:2048].reshape(D, 4, 256)
    v = bwi[:, 2048:4096].reshape(D, 4, 512)
    g = bwi[:, 4096:6144].reshape(D, 4, 512)
    b_w_in = np.ascontiguousarray(np.concatenate([q, k, v, g], 2))
    rtab, dtab = _consts()
    return {
        "cvec": cvec, "convw": convw, "rtab": rtab, "dtab": dtab,
        "a_w_in": a_w_in, "a_w_out": f(inp["a_w_out"])[0],
        "b_w_in": b_w_in, "b_w_out": f(inp["b_w_out"])[0],
        "f_w_up": f(inp["f_w_up"]), "f_w_down": f(inp["f_w_down"]),
    }


LAUNCH_PLAN = [["dsa", "ffn0", "ret", "ffn1"]]
_NC_CACHE = {}


def kernel(**inputs):
    x = np.ascontiguousarray(np.asarray(inputs["x"], dtype=np.float32))
    shared = _prep_shared(inputs)
    n = 8
    cur = [x[c] for c in range(n)]
    for phases in LAUNCH_PLAN:
        key = tuple(phases)
        if key not in _NC_CACHE:
            _NC_CACHE[key] = build(phases)
        nc = _NC_CACHE[key]
        in_maps = []
        need = needed_inputs(phases)
        for c in range(n):
            m = {k: v for k, v in shared.items() if k in need}
            m["x"] = np.ascontiguousarray(cur[c])
            in_maps.append(m)
        res = run_bass_kernel_spmd(nc, in_maps, core_ids=list(range(n)))
        cur = [np.asarray(res.results[c]["out"], dtype=np.float32) for c in range(n)]
    return np.stack(cur, 0).astype(np.float32)
```

```python
from contextlib import ExitStack

import numpy as np
import concourse.bass as bass
import concourse.mybir as mybir
from concourse.bass_utils import run_bass_kernel_spmd

F32 = mybir.dt.float32
BF16 = mybir.dt.bfloat16
AF = mybir.ActivationFunctionType
ALU = mybir.AluOpType
AX = mybir.AxisListType

ENGS = ("pe", "dve", "act", "pool", "sp")
NDMASEM = 8


class T:
    __slots__ = ("ap", "lw", "rd", "name", "lwd")

    def __init__(self, ap, name=""):
        self.ap = ap
        self.lw = None
        self.rd = {}
        self.lwd = []
        self.name = name

    def __getitem__(self, idx):
        return V(self, self.ap[idx])

    def v(self, ap=None):
        return V(self, self.ap if ap is None else ap)


class V:
    __slots__ = ("ts", "ap")

    def __init__(self, t, ap):
        self.ts = t if isinstance(t, (list, tuple)) else [t]
        self.ap = ap

    def __getitem__(self, idx):
        return V(self.ts, self.ap[idx])

    def re(self, s, **kw):
        return V(self.ts, self.ap.rearrange(s, **kw))

    def bc(self, shape):
        return V(self.ts, self.ap.to_broadcast(shape))


class Op:
    __slots__ = ("eng", "pos", "fn", "deps", "sig", "dma", "dkey", "dval", "vc", "semval", "tag")


class Prog:
    def __init__(self, nc):
        self.nc = nc
        self.ops = {e: [] for e in ENGS}
        self.clk = {e: {} for e in ENGS}
        self.ndma = {e: 0 for e in ENGS}
        self.dma_last = {}

    def _need(self, op, p, clk):
        if p is None or p is op:
            return
        if p.dma:
            key, val = p.dkey, p.dval
        else:
            key, val = p.eng, p.pos
        if clk.get(key, 0) >= val:
            return
        op.deps.append(p)
        p.sig = True
        for k, v in p.vc.items():
            if clk.get(k, 0) < v:
                clk[k] = v

    def add(self, eng, fn, reads=(), writes=(), dma=False):
        op = Op()
        op.eng = eng
        op.fn = fn
        op.tag = ''
        op.deps = []
        op.sig = False
        op.dma = dma
        op.pos = len(self.ops[eng]) + 1
        clk = self.clk[eng]
        for t in reads:
            self._need(op, t.lw, clk)
            for w in t.lwd:
                self._need(op, w, clk)
        for t in writes:
            w = t.lw
            if dma and w is not None and w.dma and w.eng == eng:
                pass
            elif w is not None and (w.dma or dma or w.eng != eng or eng != "pe"):
                self._need(op, w, clk)
            if not (dma and w is not None and w.dma and w.eng == eng):
                for w2 in t.lwd:
                    self._need(op, w2, clk)
            for k, r in t.rd.items():
                if r.dma or dma or r.eng != eng or eng != "pe":
                    self._need(op, r, clk)
        if dma:
            j = self.ndma[eng]
            self.ndma[eng] = j + 1
            slot = j % NDMASEM
            prev = self.dma_last.get((eng, slot))
            if prev is not None:
                self._need(op, prev, clk)
            op.dkey = ("dma", eng, slot)
            op.dval = 16 * (j // NDMASEM + 1)
            self.dma_last[(eng, slot)] = op
            op.vc = dict(clk)
            op.vc[op.dkey] = op.dval
        else:
            op.vc = dict(clk)
            op.vc[eng] = op.pos
        for t in reads:
            if dma:
                t.rd[("dma", eng, op.pos)] = op
            else:
                t.rd[eng] = op
        for t in writes:
            if dma and t.lw is not None and t.lw.dma and t.lw.eng == eng:
                if t.rd:
                    t.lwd = []
                else:
                    t.lwd.append(t.lw)
            else:
                t.lwd = []
            t.lw = op
            t.rd = {}
        self.ops[eng].append(op)
        return op

    def barrier(self):
        lasts = []
        for e in ENGS:
            for o in reversed(self.ops[e]):
                if not o.dma and o.fn is not None:
                    lasts.append(o)
                    break
        dmas = list(self.dma_last.values())
        for e in ENGS:
            op = Op()
            op.eng = e
            op.fn = None
            op.tag = 'barrier'
            op.deps = []
            op.sig = False
            op.dma = False
            op.pos = len(self.ops[e]) + 1
            clk = self.clk[e]
            for p in lasts + dmas:
                if p.eng == e and not p.dma:
                    continue
                self._need(op, p, clk)
            op.vc = dict(clk)
            op.vc[e] = op.pos
            self.ops[e].append(op)

    _W = ("out", "accum_out", "ap")

    def I(self, eng, method, *args, **kw):
        reads, writes, a = [], [], {}
        for k, v in kw.items():
            if isinstance(v, V):
                (writes if k in self._W else reads).extend(v.ts)
                a[k] = v.ap
            else:
                a[k] = v
        pa = []
        for v in args:
            assert not isinstance(v, V)
            pa.append(v)
        op = self.add(eng, lambda e: getattr(e, method)(*pa, **a), reads, writes)
        op.tag = method + ' ' + ','.join(t.name for t in writes) + ' <- ' + ','.join(t.name for t in reads)
        return op

    def dma(self, eng, out, in_, **kw):
        reads, writes = [], []
        if isinstance(out, V):
            writes.extend(out.ts)
            out = out.ap
        if isinstance(in_, V):
            reads.extend(in_.ts)
            in_ = in_.ap
        return self.add(eng, lambda e: e.dma_start(out=out, in_=in_, **kw), reads, writes, dma=True)

    def emit(self, stack):
        nc = self.nc
        sem = {e: stack.enter_context(nc.semaphore("s_" + e)) for e in ENGS}
        dsem = {}
        for e in ENGS:
            if self.ndma[e]:
                for s in range(min(NDMASEM, self.ndma[e])):
                    dsem[("dma", e, s)] = stack.enter_context(nc.semaphore("d_%s%d" % (e, s)))
        for e in ENGS:
            c = 0
            for o in self.ops[e]:
                if o.sig and not o.dma:
                    c += 1
                o.semval = c
        block = stack.enter_context(nc.Block())

        def run(e, name):
            last = {}
            for o in self.ops[name]:
                for p in o.deps:
                    if p.dma:
                        e.wait_ge(dsem[p.dkey], p.dval)
                    else:
                        e.wait_ge(sem[p.eng], p.semval)
                if o.fn is None:
                    continue
                ins = o.fn(e)
                if o.dma:
                    ins.then_inc(dsem[o.dkey], 16)
                    last[o.dkey] = o.dval
                elif o.sig:
                    ins.then_inc(sem[name], 1)
            for k, v in last.items():
                e.wait_ge(dsem[k], v)

        @block.tensor
        def _(e):
            run(e, "pe")

        @block.vector
        def _(e):
            run(e, "dve")

        @block.scalar
        def _(e):
            run(e, "act")

        @block.gpsimd
        def _(e):
            run(e, "pool")

        @block.sync
        def _(e):
            run(e, "sp")

S = 2048
D = 1024
NT = 16
EPS = 1e-6
DFF = 2816
NPAIR = 22
NIT = 16
DBG = {}
import os as _os, json as _json
if _os.environ.get('KDBG'):
    DBG.update(_json.loads(_os.environ['KDBG']))
ARENA_BYTES = DBG.get('arena_kb', 196) * 1024
FFN_PARTS = [(0, 6), (6, 6), (12, 5), (17, 5)]
CV_MIX = 0
CV_FFN = 2048
CV_QG = 4096
CV_KG = 4224
CV_IQG = 4352
CV_IKG = 4416
CV_OG = 4480
NCV = 6528


def _dsize(dt):
    return 4 if dt == F32 else 2


class Arena:
    def __init__(self, ap_f32, nbytes):
        self.ap = ap_f32
        self.n = nbytes
        self.off = 0

    def alloc_ap(self, shape, dt):
        free = 1
        for s in shape[1:]:
            free *= s
        nb = free * _dsize(dt)
        al = 64 if nb >= 256 else 4
        nb_al = (nb + al - 1) // al * al
        assert self.off + nb_al <= self.n, ("arena overflow", self.off, nb_al, self.n)
        self.hi = max(getattr(self, "hi", 0), self.off + nb_al)
        v = self.ap[:, self.off // 4:(self.off + nb_al) // 4]
        if dt != F32:
            v = v.bitcast(dt)
        v = v[:, 0:free]
        self.off += nb_al
        if len(shape) == 3:
            v = v.rearrange("p (a b) -> p a b", a=shape[1])
        elif len(shape) == 4:
            v = v.rearrange("p (a b c) -> p a b c", a=shape[1], b=shape[2])
        return v

    def tile(self, name, shape, dt):
        return T(self.alloc_ap(shape, dt), name)


NEED = {"dsa": {"a_w_in", "a_w_out"}, "ffn0": {"convw", "f_w_up", "f_w_down"}, "ffn1": {"convw", "f_w_up", "f_w_down"},
        "ret": {"rtab", "dtab", "b_w_in", "b_w_out"}}


def needed_inputs(phases):
    need = {"x", "cvec"}
    for ph in phases:
        need |= NEED[ph]
    return need


def build(phases, load=True, store=True):
    nc = bass.Bass("TRN2", target_bir_lowering=False)
    dr = {}

    need = {"x", "cvec"}
    for ph in phases:
        need |= NEED[ph]

    def din(name, shape):
        if name in need:
            dr[name] = nc.dram_tensor(name, list(shape), F32, kind="ExternalInput").ap()

    din("x", [S, D])
    din("cvec", [128, NCV])
    din("convw", [2, 128, 176])
    din("rtab", [128, 2, NT, 128])
    din("dtab", [128, 8])
    din("a_w_in", [D, 1736])
    din("a_w_out", [D, D])
    din("b_w_in", [D, 4, 1536])
    din("b_w_out", [2048, D])
    din("f_w_up", [2, D, 2 * DFF])
    din("f_w_down", [2, DFF, D])
    out_d = nc.dram_tensor("out", [S, D], F32, kind="ExternalOutput").ap()

    with ExitStack() as es:
        arena_h = es.enter_context(nc.sbuf_tensor("arena", [128, ARENA_BYTES // 4], F32))
        AR = Arena(arena_h.ap(), ARENA_BYTES)
        banks = [T(es.enter_context(nc.psum_tensor("bank%d" % i, [128, 512], F32)).ap(), "bank%d" % i)
                 for i in range(8)]
        P = Prog(nc)

        def bf(bank):
            return V(bank, bank.ap.bitcast(BF16))

        if DBG.get('pad_kb'):
            AR.alloc_ap([128, DBG['pad_kb'] * 256], F32)
        xs_ap = AR.alloc_ap([128, NT, D], F32)
        xs = [T(xs_ap[:, i, :], "x%d" % i) for i in range(NT)]
        ident = AR.tile("ident", [128, 128], BF16)
        ones = AR.tile("ones", [128, 128], BF16)
        cmaskT = AR.tile("cmaskT", [128, 128], BF16)
        P.I("pool", "memset", ap=ident[:], constant=1.0)
        P.I("pool", "affine_select", out=ident[:], in_=ident[:], pattern=[[-1, 128]],
            compare_op=ALU.is_equal, fill=0.0, base=0, channel_multiplier=1)
        P.I("pool", "memset", ap=ones[:], constant=1.0)
        P.I("pool", "memset", ap=cmaskT[:], constant=1.0)
        P.I("pool", "affine_select", out=cmaskT[:], in_=cmaskT[:], pattern=[[1, 128]],
            compare_op=ALU.is_ge, fill=0.0, base=0, channel_multiplier=-1)
        base_mark = AR.off

        x_v = dr["x"].rearrange("(i p) d -> p i d", p=128)
        o_v = out_d.rearrange("(i p) d -> p i d", p=128)
        if load:
            for i in range(NT):
                P.dma("sp", xs[i][:], x_v[:, i, :])

        def norm_T(G, hT_ap, hT_t, tb):
            ss = [AR.tile("ss%d" % i, [128, 1], F32) for i in range(NT)]
            sd = [AR.tile("sd%d" % i, [128, 1], F32) for i in range(NT)]
            rs = [AR.tile("rs%d" % i, [128, 1], F32) for i in range(NT)]
            hb = [AR.tile("hb%d" % k, [128, D], BF16) for k in range(2)]
            for i in range(NT):
                P.I("dve", "memset", ap=ss[i][:], constant=0.0)
                P.I("act", "activation", out=hb[i % 2][:], in_=xs[i][:], func=AF.Square, accum_out=ss[i][:])
                P.I("act", "activation", out=sd[i][:], in_=ss[i][:], func=AF.Sqrt, bias=EPS, scale=1.0 / D)
                P.I("dve", "reciprocal", out=rs[i][:], in_=sd[i][:])
                h = hb[i % 2]
                P.I("dve", "scalar_tensor_tensor", out=h[:], in0=xs[i][:], scalar=rs[i][:], in1=G[:],
                    op0=ALU.mult, op1=ALU.mult)
                bk = banks[tb[i % len(tb)]]
                bkv = bf(bk)
                for c in range(8):
                    P.I("pe", "transpose", out=bkv[:, c * 128:(c + 1) * 128], in_=h[:, c * 128:(c + 1) * 128],
                        identity=ident[:])
                P.I("act", "activation", out=V(hT_t[i], hT_ap[:, :, i * 128:(i + 1) * 128]),
                    in_=bkv.re("p (c t) -> p c t", c=8), func=AF.Copy)

        def alloc_hT():
            hT_ap = AR.alloc_ap([128, 8, S], BF16)
            hT_t = [T(hT_ap[:, :, i * 128:(i + 1) * 128], "hT%d" % i) for i in range(NT)]
            return hT_ap, hT_t

        def phase_ffn(l):
            cw = AR.tile("cw", [128, 4, 44], F32)
            P.dma("sp", cw[:], dr["convw"][l].rearrange("p (a b) -> p a b", a=4))
            hT_ap, hT_t = alloc_hT()
            wu = [AR.tile("wu%d" % k, [128, 8, 2, 6 * 128], BF16) for k in range(2)]
            wd = [AR.tile("wd%d" % k, [128, 6, D], BF16) for k in range(2)]
            ffn_mark = AR.off
            G = AR.tile("G", [128, D], F32)
            P.dma("sp", G[:], dr["cvec"][:, CV_FFN + l * D: CV_FFN + (l + 1) * D])
            wup_v = dr["f_w_up"][l].rearrange("(kc p) n -> p kc n", p=128)
            wdn_v = dr["f_w_down"][l].rearrange("(c p) n -> p c n", p=128)

            def load_part(k):
                p0, npp = FFN_PARTS[k]
                for hf in range(2):
                    for kq in range(4):
                        P.dma("pool", wu[k % 2][:, 2 * kq:2 * kq + 2, hf, 0:npp * 128],
                              wup_v[:, 2 * kq:2 * kq + 2, hf * DFF + p0 * 128: hf * DFF + (p0 + npp) * 128])
                for j0 in range(0, npp, 3):
                    j1 = min(npp, j0 + 3)
                    P.dma("pool", wd[k % 2][:, j0:j1, :], wdn_v[:, p0 + j0:p0 + j1, :])

            load_part(0)
            norm_T(G, hT_ap, hT_t, [6, 7])
            P.barrier()
            AR.off = ffn_mark
            gT = [AR.tile("gT%d" % k, [128, 6, 512], BF16) for k in range(2)]
            va = [AR.tile("va%d" % k, [128, 512], F32) for k in range(2)]
            vb = [AR.tile("vb%d" % k, [128, 512], F32) for k in range(2)]
            halo = [AR.tile("halo%d" % k, [128, 2, 6, 2], F32) for k in range(2)]
            upb = 0
            dnb = 0
            cnt = 0
            for k in range(DBG.get("ffn_parts", len(FFN_PARTS))):
                p0, npp = FFN_PARTS[k]
                if k + 1 < len(FFN_PARTS):
                    load_part(k + 1)
                wuk, wdk = wu[k % 2], wd[k % 2]
                for g in range(4):
                    hTg = V(hT_t[4 * g:4 * g + 4], hT_ap[:, :, g * 512:(g + 1) * 512])
                    gt = gT[cnt % 2]
                    cnt += 1
                    hcur, hprev = halo[g % 2], halo[(g + 1) % 2]
                    for j in range(npp):
                        vv = [va[j % 2], vb[j % 2]]
                        pss = []
                        for hf in range(2):
                            ps = banks[upb % 4]
                            upb += 1
                            pss.append(ps)
                            for kc in range(8):
                                P.I("pe", "matmul", out=ps[:], lhsT=wuk[:, kc, hf, j * 128:(j + 1) * 128],
                                    rhs=hTg[:, kc, :], start=(kc == 0), stop=(kc == 7))
                        SK = DBG.get("skip", ())
                        for hf in range(2):
                            cc = hf * NPAIR + p0 + j
                            P.I("act", "activation", out=vv[hf][:], in_=pss[hf][:], func=AF.Identity,
                                bias=cw[:, 3, cc:cc + 1], scale=cw[:, 2, cc:cc + 1])
                        if "tap" not in SK:
                            for hf in range(2):
                                cc = hf * NPAIR + p0 + j
                                P.I("dve", "scalar_tensor_tensor", out=vv[hf][:, 1:512], in0=pss[hf][:, 0:511],
                                    scalar=cw[:, 1, cc:cc + 1], in1=vv[hf][:, 1:512], op0=ALU.mult, op1=ALU.add)
                            for hf in range(2):
                                cc = hf * NPAIR + p0 + j
                                P.I("dve", "scalar_tensor_tensor", out=vv[hf][:, 2:512], in0=pss[hf][:, 0:510],
                                    scalar=cw[:, 0, cc:cc + 1], in1=vv[hf][:, 2:512], op0=ALU.mult, op1=ALU.add)
                        if "halo" not in SK:
                            if "halo_act" not in SK:
                              for hf in range(2):
                                if DBG.get("halo_on_dve", 1):
                                    P.I("dve", "tensor_copy", out=hcur[:, hf, j, :], in_=pss[hf][:, 510:512])
                                else:
                                    P.I("act", "activation", out=hcur[:, hf, j, :], in_=pss[hf][:, 510:512], func=AF.Copy)
                            if g > 0 and "halo_dve" not in SK:
                                for hf in range(2):
                                    cc = hf * NPAIR + p0 + j
                                    P.I("dve", "scalar_tensor_tensor", out=vv[hf][:, 0:1], in0=hprev[:, hf, j, 1:2],
                                        scalar=cw[:, 1, cc:cc + 1], in1=vv[hf][:, 0:1], op0=ALU.mult, op1=ALU.add)
                                for hf in range(2):
                                    cc = hf * NPAIR + p0 + j
                                    P.I("dve", "scalar_tensor_tensor", out=vv[hf][:, 0:2], in0=hprev[:, hf, j, 0:2],
                                        scalar=cw[:, 0, cc:cc + 1], in1=vv[hf][:, 0:2], op0=ALU.mult, op1=ALU.add)
                        if "silu" not in SK:
                            P.I("act", "activation", out=vv[0][:], in_=vv[0][:], func=AF.Silu)
                        if "gate" not in SK:
                            P.I(DBG.get("gate_eng", "dve"), "tensor_tensor", out=gt[:, j, :], in0=vv[0][:], in1=vv[1][:], op=ALU.mult)
                        else:
                            P.I("dve", "tensor_copy", out=gt[:, j, :], in_=vv[0][:])
                    if "down" in DBG.get("skip", ()):
                        continue
                    for tt in range(4):
                        xi = xs[4 * g + tt]
                        for dh in range(2):
                            ps = banks[4 + dnb % 4]
                            dnb += 1
                            for j in range(npp):
                                P.I("pe", "matmul", out=ps[:], lhsT=gt[:, j, tt * 128:(tt + 1) * 128],
                                    rhs=wdk[:, j, dh * 512:(dh + 1) * 512], start=(j == 0), stop=(j == npp - 1))
                            P.I("dve", "tensor_tensor", out=xi[:, dh * 512:(dh + 1) * 512],
                                in0=xi[:, dh * 512:(dh + 1) * 512], in1=ps[:], op=ALU.add)

        def phase_dsa():
            cv = dr["cvec"]
            gq = AR.tile("gq", [128, 128], F32)
            gk = AR.tile("gk", [128, 128], F32)
            giq = AR.tile("giq", [128, 64], F32)
            gik = AR.tile("gik", [128, 64], F32)
            P.dma("sp", gq[:], cv[:, CV_QG:CV_QG + 128])
            P.dma("sp", gk[:], cv[:, CV_KG:CV_KG + 128])
            P.dma("sp", giq[:], cv[:, CV_IQG:CV_IQG + 64])
            P.dma("sp", gik[:], cv[:, CV_IKG:CV_IKG + 64])
            qT_ap = AR.alloc_ap([128, 8, S], BF16)
            qT = [T(qT_ap[:, :, i * 128:(i + 1) * 128], "qT%d" % i) for i in range(NT)]
            qiT_ap = AR.alloc_ap([128, 4, S], BF16)
            qiT = [T(qiT_ap[:, :, i * 128:(i + 1) * 128], "qiT%d" % i) for i in range(NT)]
            kT_ap = AR.alloc_ap([128, S], BF16)
            kT = [T(kT_ap[:, i * 128:(i + 1) * 128], "kT%d" % i) for i in range(NT)]
            kiT_ap = AR.alloc_ap([128, S], BF16)
            kiT = [T(kiT_ap[:, i * 128:(i + 1) * 128], "kiT%d" % i) for i in range(NT)]
            c_ap = AR.alloc_ap([128, NT, 128], BF16)
            c_t = [T(c_ap[:, i, :], "c%d" % i) for i in range(NT)]
            wi_ap = AR.alloc_ap([128, NT, 8], F32)
            wi_t = [T(wi_ap[:, i, :], "wi%d" % i) for i in range(NT)]
            nb = AR.tile("nbias", [128, 1], F32)
            pw2 = AR.tile("pw2", [128, NIT + 1], F32)
            for it in range(NIT + 1):
                P.I("pool", "memset", ap=pw2[:, it:it + 1], constant=2.0 ** -(it + 1))
            t1 = AR.tile("t1", [128, 128], F32)
            m1 = AR.tile("m1", [128, 1], F32)
            m2 = AR.tile("m2", [128, 1], F32)
            P.I("dve", "tensor_tensor", out=t1[:], in0=gq[:], in1=gq[:], op=ALU.mult)
            P.I("dve", "tensor_reduce", out=m1[:], in_=t1[:], axis=AX.X, op=ALU.max)
            P.I("dve", "tensor_tensor", out=t1[:], in0=gk[:], in1=gk[:], op=ALU.mult)
            P.I("dve", "tensor_reduce", out=m2[:], in_=t1[:], axis=AX.X, op=ALU.max)
            P.I("dve", "tensor_tensor", out=m1[:], in0=m1[:], in1=m2[:], op=ALU.mult)
            P.I("act", "activation", out=m2[:], in_=m1[:], func=AF.Sqrt, scale=128.0)
            P.I("dve", "tensor_scalar", out=nb[:], in0=m2[:], scalar1=-1.0, scalar2=None, op0=ALU.mult)
            mark1 = AR.off

            hT_ap, hT_t = alloc_hT()
            w_in = AR.tile("w_in", [128, 8, 1736], BF16)
            wv = dr["a_w_in"].rearrange("(kc p) n -> p kc n", p=128)
            for kq in range(4):
                P.dma("pool", w_in[:, 2 * kq:2 * kq + 2, :], wv[:, 2 * kq:2 * kq + 2, :])
            mark_n = AR.off
            G = AR.tile("G", [128, D], F32)
            P.dma("sp", G[:], cv[:, CV_MIX:CV_MIX + D])
            norm_T(G, hT_ap, hT_t, [6, 7])
            P.barrier()
            AR.off = mark_n
            ssq = [AR.tile("ssq%d" % k, [128, 18], F32) for k in range(2)]
            sdq = [AR.tile("sdq%d" % k, [128, 18], F32) for k in range(2)]
            rsq = [AR.tile("rsq%d" % k, [128, 18], F32) for k in range(2)]
            sjunk = AR.tile("sjunk", [128, 128], BF16)
            qn = [AR.tile("qn%d" % k, [128, 1024], BF16) for k in range(2)]
            qin = [AR.tile("qin%d" % k, [128, 512], BF16) for k in range(2)]
            kn = [AR.tile("kn%d" % k, [128, 128], BF16) for k in range(2)]
            kin = [AR.tile("kin%d" % k, [128, 128], BF16) for k in range(2)]
            colb = [(0, 512), (512, 1024), (1024, 1536), (1536, 1736)]
            S1 = DBG.get('s1_sub', 'msncte')
            for i in range(DBG.get('s1_tiles', NT)):
                for bi, (c0, c1) in enumerate(colb):
                    for kc in range(8):
                        P.I("pe", "matmul", out=banks[bi][:, 0:c1 - c0], lhsT=hT_t[i].v(hT_ap[:, kc, i * 128:(i + 1) * 128]),
                            rhs=w_in[:, kc, c0:c1], start=(kc == 0), stop=(kc == 7))
                if 's' not in S1:
                    continue
                sq, sdd, rr = ssq[i % 2], sdq[i % 2], rsq[i % 2]
                P.I("dve", "memset", ap=sq[:], constant=0.0)
                for h in range(8):
                    P.I("act", "activation", out=sjunk[:, 0:128], in_=banks[h // 4][:, (h % 4) * 128:(h % 4 + 1) * 128],
                        func=AF.Square, accum_out=sq[:, h:h + 1])
                P.I("act", "activation", out=sjunk[:, 0:128], in_=banks[3][:, 0:128], func=AF.Square,
                    accum_out=sq[:, 8:9])
                for h in range(8):
                    P.I("act", "activation", out=sjunk[:, 0:64], in_=banks[2][:, h * 64:(h + 1) * 64],
                        func=AF.Square, accum_out=sq[:, 9 + h:10 + h])
                P.I("act", "activation", out=sjunk[:, 0:64], in_=banks[3][:, 128:192], func=AF.Square,
                    accum_out=sq[:, 17:18])
                P.I("act", "activation", out=sdd[:, 0:9], in_=sq[:, 0:9], func=AF.Sqrt, bias=EPS, scale=1.0 / 128)
                P.I("act", "activation", out=sdd[:, 9:18], in_=sq[:, 9:18], func=AF.Sqrt, bias=EPS, scale=1.0 / 64)
                P.I("dve", "reciprocal", out=rr[:], in_=sdd[:])
                if 'n' not in S1:
                    continue
                qn_, qin_, kn_, kin_ = qn[i % 2], qin[i % 2], kn[i % 2], kin[i % 2]
                for h in range(8):
                    P.I("dve", "scalar_tensor_tensor", out=qn_[:, h * 128:(h + 1) * 128],
                        in0=banks[h // 4][:, (h % 4) * 128:(h % 4 + 1) * 128], scalar=rr[:, h:h + 1], in1=gq[:],
                        op0=ALU.mult, op1=ALU.mult)
                for h in range(8):
                    P.I("dve", "scalar_tensor_tensor", out=qin_[:, h * 64:(h + 1) * 64],
                        in0=banks[2][:, h * 64:(h + 1) * 64], scalar=rr[:, 9 + h:10 + h], in1=giq[:],
                        op0=ALU.mult, op1=ALU.mult)
                P.I("dve", "scalar_tensor_tensor", out=kn_[:], in0=banks[3][:, 0:128], scalar=rr[:, 8:9], in1=gk[:],
                    op0=ALU.mult, op1=ALU.mult)
                for r in range(2):
                    P.I("dve", "scalar_tensor_tensor", out=kin_[:, r * 64:(r + 1) * 64], in0=banks[3][:, 128:192],
                        scalar=rr[:, 17:18], in1=gik[:], op0=ALU.mult, op1=ALU.mult)
                if 'c' not in S1:
                    continue
                P.I("act", "activation", out=c_t[i][:], in_=banks[3][:, 0:128], func=AF.Copy)
                P.I("act", "activation", out=wi_t[i][:], in_=banks[3][:, 192:200], func=AF.Identity,
                    scale=float(8 ** -0.5 * 64 ** -0.5))
                if 't' not in S1:
                    continue
                tb0 = bf(banks[4 + 2 * (i % 2)])
                tb1 = bf(banks[5 + 2 * (i % 2)])
                for h in range(8):
                    P.I("pe", "transpose", out=tb0[:, h * 128:(h + 1) * 128], in_=qn_[:, h * 128:(h + 1) * 128],
                        identity=ident[:])
                for r in range(4):
                    P.I("pe", "transpose", out=tb1[:, r * 128:(r + 1) * 128], in_=qin_[:, r * 128:(r + 1) * 128],
                        identity=ident[:])
                P.I("pe", "transpose", out=tb1[:, 512:640], in_=kn_[:], identity=ident[:])
                P.I("pe", "transpose", out=tb1[:, 640:768], in_=kin_[:], identity=ident[:])
                if 'e' not in S1:
                    continue
                P.I("act", "activation", out=qT[i][:], in_=tb0.re("p (c t) -> p c t", c=8), func=AF.Copy)
                P.I("act", "activation", out=qiT[i][:], in_=tb1[:, 0:512].re("p (c t) -> p c t", c=4), func=AF.Copy)
                P.I("act", "activation", out=kT[i][:], in_=tb1[:, 512:640], func=AF.Copy)
                P.I("act", "activation", out=kiT[i][:], in_=tb1[:, 640:768], func=AF.Copy)
            P.barrier()
            AR.off = mark1
            if DBG.get("dsa_stage", 9) < 2:
                return

            w_out = AR.tile("w_out", [128, 8, D], BF16)
            wov = dr["a_w_out"].rearrange("(h p) n -> p h n", p=128)
            for kq in range(4):
                P.dma("pool", w_out[:, 2 * kq:2 * kq + 2, :], wov[:, 2 * kq:2 * kq + 2, :])
            score = [AR.tile("score%d" % k, [128, S], F32) for k in range(2)]
            rel = [AR.tile("rel%d" % k, [128, 512], F32) for k in range(2)]
            cjunk = AR.tile("cjunk", [128, S], BF16)
            maskq = AR.tile("maskq", [128, S], BF16)
            maskT = AR.tile("maskT", [128, NT, 128], BF16)
            PT = [AR.tile("PT%d" % k, [128, 512], BF16) for k in range(3)]
            oT = [AR.tile("oT%d" % k, [128, 8, 128], BF16) for k in range(2)]
            rden = [AR.tile("rden%d" % k, [128, 512], F32) for k in range(2)]
            sm = [[AR.tile("sm%d_%d" % (k, q), [128, 1], F32) for q in range(6)] for k in range(2)]
            wtab = [AR.tile("wtab%d" % k, [128, NIT + 1], F32) for k in range(2)]
            maskT2 = [maskT, AR.tile("maskTb", [128, NT, 128], BF16)]
            lnd = [AR.tile("lnd%d" % k, [128, 512], F32) for k in range(2)]
            cnts = {"ib": 0, "lb": 0, "pb": 0}

            def gen_A(b):
                nk = (b + 1) * 128
                sc = score[b % 2]
                mx, mn, w0, thr, cn, dd = sm[b % 2]
                wt = wtab[b % 2]
                mT = maskT2[b % 2]
                nch = (nk + 511) // 512
                for h in range(8):
                    r0 = (h % 2) * 64
                    for kc in range(nch):
                        c0, c1 = kc * 512, min(nk, (kc + 1) * 512)
                        ps = banks[cnts["ib"] % 2]
                        rl = rel[cnts["ib"] % 2]
                        cnts["ib"] += 1
                        kts = kiT[c0 // 128:c1 // 128]
                        P.I("pe", "matmul", out=ps[:, 0:c1 - c0], lhsT=qiT[b].v(qiT_ap[r0:r0 + 64, h // 2, b * 128:(b + 1) * 128]),
                            rhs=V(kts, kiT_ap[r0:r0 + 64, c0:c1]), start=True, stop=True)
                        P.I("act", "activation", out=rl[:, 0:c1 - c0], in_=ps[:, 0:c1 - c0], func=AF.Relu)
                        IE = DBG.get("idx_eng", "dve")
                        if h == 0:
                            P.I(IE, "tensor_scalar", out=sc[:, c0:c1], in0=rl[:, 0:c1 - c0],
                                scalar1=wi_t[b].v(wi_ap[:, b, 0:1]), scalar2=None, op0=ALU.mult)
                        elif IE == "dve":
                            P.I(IE, "scalar_tensor_tensor", out=sc[:, c0:c1], in0=rl[:, 0:c1 - c0],
                                scalar=wi_t[b].v(wi_ap[:, b, h:h + 1]), in1=sc[:, c0:c1], op0=ALU.mult, op1=ALU.add)
                        else:
                            P.I(IE, "tensor_scalar", out=rl[:, 0:c1 - c0], in0=rl[:, 0:c1 - c0],
                                scalar1=wi_t[b].v(wi_ap[:, b, h:h + 1]), scalar2=None, op0=ALU.mult)
                            P.I(IE, "tensor_tensor", out=sc[:, c0:c1], in0=sc[:, c0:c1], in1=rl[:, 0:c1 - c0], op=ALU.add)
                        yield
                if b >= 2:
                    P.I("dve", "tensor_reduce", out=mx[:], in_=sc[:, 0:nk], axis=AX.X, op=ALU.max)
                    P.I("dve", "tensor_reduce", out=mn[:], in_=sc[:, 0:nk], axis=AX.X, op=ALU.min)
                P.I("dve", "memset", ap=sc[0:64, nk - 64:nk], constant=-1e30)
                yield
                if b >= 2:
                    P.I("dve", "tensor_scalar", out=w0[:], in0=mx[:], scalar1=mn[:], scalar2=1.001,
                        op0=ALU.subtract, op1=ALU.mult)
                    P.I("dve", "tensor_scalar", out=wt[:], in0=pw2[:], scalar1=w0[:], scalar2=None, op0=ALU.mult)
                    P.I("dve", "tensor_tensor", out=thr[:], in0=mn[:], in1=wt[:, 0:1], op=ALU.add)
                    for it in range(NIT):
                        P.I("dve", "memset", ap=cn[:], constant=0.0)
                        P.I("dve", "tensor_scalar", out=cjunk[:, 0:nk], in0=sc[:, 0:nk], scalar1=thr[:], scalar2=0.0,
                            op0=ALU.is_ge, op1=ALU.add, accum_out=cn[:])
                        yield
                        P.I("dve", "tensor_scalar", out=dd[:], in0=cn[:], scalar1=255.5, scalar2=0.5,
                            op0=ALU.is_ge, op1=ALU.subtract)
                        P.I("dve", "scalar_tensor_tensor", out=thr[:], in0=dd[:], scalar=wt[:, it:it + 1], in1=thr[:],
                            op0=ALU.mult, op1=ALU.add)
                        yield
                    P.I("dve", "tensor_tensor", out=thr[:], in0=thr[:], in1=wt[:, NIT:NIT + 1], op=ALU.subtract)
                else:
                    P.I("dve", "memset", ap=thr[:], constant=-1e29)
                P.I("dve", "tensor_scalar", out=maskq[:, 0:nk], in0=sc[:, 0:nk], scalar1=thr[:], scalar2=None,
                    op0=ALU.is_ge)
                yield
                bm = bf(banks[4])
                for j0 in range(0, b + 1, 8):
                    j1 = min(b + 1, j0 + 8)
                    for j in range(j0, j1):
                        P.I("pe", "transpose", out=bm[:, (j - j0) * 128:(j - j0 + 1) * 128],
                            in_=maskq[:, j * 128:(j + 1) * 128], identity=ident[:])
                    P.I("act", "activation", out=mT[:, j0:j1, :],
                        in_=bm[:, 0:(j1 - j0) * 128].re("p (c t) -> p c t", c=j1 - j0), func=AF.Copy)
                    yield

            def gen_B(b):
                mT = maskT2[b % 2]
                o_ = oT[b % 2]
                for hh in range(2):
                    rd = rden[hh]
                    for j in range(b + 1):
                        pl = banks[2 + cnts["lb"] % 2]
                        cnts["lb"] += 1
                        pt = PT[cnts["pb"] % 3]
                        cnts["pb"] += 1
                        P.I("pe", "matmul", out=pl[:], lhsT=kT[j][:], rhs=qT[b].v(qT_ap[:, hh * 4:(hh + 1) * 4, b * 128:(b + 1) * 128]),
                            start=True, stop=True)
                        P.I("act", "activation", out=pt[:], in_=pl[:], func=AF.Exp, bias=nb[:], scale=float(128 ** -0.5))
                        P.I("dve", "tensor_tensor", out=pt[:].re("p (h q) -> p h q", h=4),
                            in0=pt[:].re("p (h q) -> p h q", h=4),
                            in1=V(mT, mT.ap[:, j, :].unsqueeze(1).to_broadcast([128, 4, 128])), op=ALU.mult)
                        P.I("pe", "matmul", out=banks[5][:], lhsT=c_t[j][:], rhs=pt[:], start=(j == 0), stop=(j == b))
                        P.I("pe", "matmul", out=banks[6][:], lhsT=ones[:], rhs=pt[:], start=(j == 0), stop=(j == b))
                        yield
                    P.I("act", "activation", out=lnd[hh][:], in_=banks[6][:], func=AF.Ln)
                    P.I("act", "activation", out=rd[:], in_=lnd[hh][:], func=AF.Exp, scale=-1.0)
                    P.I("dve", "tensor_tensor", out=o_[:, hh * 4:(hh + 1) * 4, :].re("p h q -> p (h q)"),
                        in0=banks[5][:], in1=rd[:], op=ALU.mult)
                    yield
                for dh in range(2):
                    for h in range(8):
                        P.I("pe", "matmul", out=banks[7][:], lhsT=o_[:, h, :], rhs=w_out[:, h, dh * 512:(dh + 1) * 512],
                            start=(h == 0), stop=(h == 7))
                    P.I("dve", "tensor_tensor", out=xs[b][:, dh * 512:(dh + 1) * 512],
                        in0=xs[b][:, dh * 512:(dh + 1) * 512], in1=banks[7][:], op=ALU.add)
                    yield

            def _drain(gs):
                gs = list(gs)
                while gs:
                    for g in list(gs):
                        try:
                            next(g)
                        except StopIteration:
                            gs.remove(g)

            NB = DBG.get('dsa_blocks', NT)
            _drain([gen_A(0)])
            for b in range(NB):
                gs = [gen_B(b)]
                if b + 1 < NB:
                    gs.append(gen_A(b + 1))
                _drain(gs)

        def phase_ret():
            cv = dr["cvec"]
            ogh = [AR.tile("ogh%d" % k, [128, 512], F32) for k in range(2)]
            rt = AR.tile("rtab", [128, 2, NT, 128], F32)
            P.dma("sp", rt[:, 0], dr["rtab"][:, 0])
            P.dma("sp", rt[:, 1], dr["rtab"][:, 1])
            dt_ = AR.tile("dtab", [128, 8], F32)
            P.dma("sp", dt_[:], dr["dtab"])
            hT_ap, hT_t = alloc_hT()
            w_in = [AR.tile("rw_in%d" % c, [128, 8, 512], BF16) for c in range(3)]
            w_out = [AR.tile("rw_out%d" % k, [128, 4, D], BF16) for k in range(2)]
            mark_n = AR.off
            G = AR.tile("G", [128, D], F32)
            P.dma("sp", G[:], cv[:, CV_MIX + D:CV_MIX + 2 * D])
            wiv = dr["b_w_in"].rearrange("(kc p) h n -> p kc h n", p=128)
            wov = dr["b_w_out"].rearrange("(h c p) n -> p h c n", p=128, c=4)

            def load_head(h):
                for c in range(3):
                    for kq in range(2):
                        P.dma("pool", w_in[c][:, 4 * kq:4 * kq + 4, :], wiv[:, 4 * kq:4 * kq + 4, h, c * 512:(c + 1) * 512])
                P.dma("pool", w_out[h % 2][:], wov[:, h])
                P.dma("sp", ogh[h % 2][:], cv[:, CV_OG + h * 512:CV_OG + (h + 1) * 512])

            load_head(0)
            norm_T(G, hT_ap, hT_t, [6, 7])
            P.barrier()
            AR.off = mark_n
            U = AR.tile("U", [128, 2, 512], F32)
            sbf = [AR.tile("sbf%d" % k, [128, 2, 512], BF16) for k in range(2)]
            qk = [AR.tile("qk%d" % k, [128, 512], F32) for k in range(2)]
            ra = [AR.tile("ra%d" % k, [128, 2, 128], F32) for k in range(1)] * 2
            rb = [AR.tile("rb%d" % k, [128, 2, 128], F32) for k in range(1)] * 2
            rc = [AR.tile("rc%d" % k, [128, 2, 128], F32) for k in range(1)] * 2
            rd_ = [AR.tile("rd%d" % k, [128, 2, 128], F32) for k in range(1)] * 2
            qkr = [AR.tile("qkr%d" % k, [128, 2, 2, 128], BF16) for k in range(2)]
            qkT = [AR.tile("qkT%d" % k, [128, 4, 128], BF16) for k in range(2)]
            vbf = [AR.tile("vbf%d" % k, [128, 512], BF16) for k in range(2)]
            ST = [AR.tile("ST%d" % k, [128, 128], BF16) for k in range(2)]
            sg = [AR.tile("sg%d" % k, [128, 512], F32) for k in range(2)]
            sg2 = sg
            yb = [AR.tile("yb%d" % k, [128, 512], BF16) for k in range(2)]
            yT = [AR.tile("yT%d" % k, [128, 4, 128], BF16) for k in range(2)]
            rsm = [[AR.tile("rsm%d_%d" % (k, q), [128, 1], F32) for q in range(3)] for k in range(2)]
            rjunk = AR.tile("rjunk", [128, 512], BF16)
            def ret_gen(h, i, k2, g128, wo):
                if i == 0 and h > 0:
                    load_head(h)
                for c in range(3):
                    for kc in range(8):
                        P.I("pe", "matmul", out=banks[c][:], lhsT=hT_t[i].v(hT_ap[:, kc, i * 128:(i + 1) * 128]),
                            rhs=w_in[c][:, kc, :], start=(kc == 0), stop=(kc == 7))
                q_ = qk[k2]
                P.I("act", "activation", out=q_[:, 0:256], in_=banks[0][:, 0:256], func=AF.Identity, scale=dt_[:, h:h + 1])
                P.I("act", "activation", out=q_[:, 256:512], in_=banks[0][:, 256:512], func=AF.Identity,
                    scale=dt_[:, 4 + h:5 + h])
                P.I("act", "activation", out=vbf[k2][:], in_=banks[1][:], func=AF.Copy)
                P.I("act", "activation", out=sg[k2][:], in_=banks[2][:], func=AF.Silu)
                P.I("dve", "tensor_tensor", out=sg2[k2][:], in0=sg[k2][:], in1=ogh[h % 2][:], op=ALU.mult)
                yield None
                qv = q_[:].re("p (a b c) -> p a b c", a=2, b=2)
                X1, X2 = qv[:, :, 0, :], qv[:, :, 1, :]
                cosb = V(rt, rt.ap[:, 0, i, :].unsqueeze(1).to_broadcast([128, 2, 128]))
                sinb = V(rt, rt.ap[:, 1, i, :].unsqueeze(1).to_broadcast([128, 2, 128]))
                P.I("dve", "tensor_tensor", out=ra[k2][:], in0=X1, in1=cosb, op=ALU.mult)
                P.I("dve", "tensor_tensor", out=rb[k2][:], in0=X2, in1=sinb, op=ALU.mult)
                P.I("dve", "tensor_tensor", out=rc[k2][:], in0=X1, in1=sinb, op=ALU.mult)
                P.I("dve", "tensor_tensor", out=rd_[k2][:], in0=X2, in1=cosb, op=ALU.mult)
                P.I("dve", "tensor_tensor", out=qkr[k2][:, :, 0, :], in0=ra[k2][:], in1=rb[k2][:], op=ALU.subtract)
                P.I("dve", "tensor_tensor", out=qkr[k2][:, :, 1, :], in0=rc[k2][:], in1=rd_[k2][:], op=ALU.add)
                yield None
                b3 = bf(banks[3])
                for a in range(2):
                    for c in range(2):
                        P.I("pe", "transpose", out=b3[:, (2 * a + c) * 128:(2 * a + c + 1) * 128],
                            in_=qkr[k2][:, a, c, :], identity=ident[:])
                P.I("act", "activation", out=qkT[k2][:], in_=b3[:, 0:512].re("p (c t) -> p c t", c=4), func=AF.Copy)
                yield None
                at = banks[3][:, 256:384]
                for c in range(2):
                    P.I("pe", "matmul", out=at, lhsT=qkT[k2][:, 2 + c, :], rhs=qkT[k2][:, c, :],
                        start=(c == 0), stop=(c == 1))
                P.I("dve", "tensor_tensor", out=ST[k2][:], in0=at, in1=cmaskT[:], op=ALU.mult)
                yield 'MID'
                P.I("pe", "matmul", out=banks[4][:], lhsT=ST[k2][:], rhs=vbf[k2][:], start=True, stop=(i == 0))
                if i > 0:
                    sb_ = sbf[(i - 1) % 2]
                    for c in range(2):
                        P.I("pe", "matmul", out=banks[4][:], lhsT=qkT[k2][:, c, :], rhs=sb_[:, c, :],
                            start=False, stop=(c == 1))
                yield None
                if i < NT - 1:
                    for c in range(2):
                        P.I("pe", "matmul", out=banks[5 + c][:], lhsT=qkr[k2][:, 1, c, :], rhs=vbf[k2][:],
                            start=True, stop=True)
                    for c in range(2):
                        if i == 0:
                            P.I("dve", "tensor_copy", out=U[:, c, :], in_=banks[5 + c][:])
                        else:
                            P.I("dve", "scalar_tensor_tensor", out=U[:, c, :], in0=U[:, c, :], scalar=g128,
                                in1=banks[5 + c][:], op0=ALU.mult, op1=ALU.add)
                    P.I("act", "activation", out=sbf[i % 2][:], in_=U[:], func=AF.Identity, scale=g128)
                yield None
                ssr, sdr, rsr = rsm[k2]
                P.I("dve", "memset", ap=ssr[:], constant=0.0)
                P.I("act", "activation", out=rjunk[:], in_=banks[4][:], func=AF.Square, accum_out=ssr[:])
                P.I("act", "activation", out=sdr[:], in_=ssr[:], func=AF.Sqrt, bias=EPS, scale=1.0 / 512)
                P.I("dve", "reciprocal", out=rsr[:], in_=sdr[:])
                P.I("dve", "scalar_tensor_tensor", out=yb[k2][:], in0=banks[4][:], scalar=rsr[:], in1=sg2[k2][:],
                    op0=ALU.mult, op1=ALU.mult)
                yield None
                b7 = bf(banks[7])
                for e in range(4):
                    P.I("pe", "transpose", out=b7[:, e * 128:(e + 1) * 128], in_=yb[k2][:, e * 128:(e + 1) * 128],
                        identity=ident[:])
                P.I("act", "activation", out=yT[k2][:], in_=b7[:, 0:512].re("p (c t) -> p c t", c=4), func=AF.Copy)
                for dh in range(2):
                    pso = banks[5 + dh]
                    for e in range(4):
                        P.I("pe", "matmul", out=pso[:], lhsT=yT[k2][:, e, :], rhs=wo[:, e, dh * 512:(dh + 1) * 512],
                            start=(e == 0), stop=(e == 3))
                    P.I("dve", "tensor_tensor", out=xs[i][:, dh * 512:(dh + 1) * 512],
                        in0=xs[i][:, dh * 512:(dh + 1) * 512], in1=pso[:], op=ALU.add)

            def _step(g):
                try:
                    return next(g), True
                except StopIteration:
                    return None, False

            gens = []
            it = 0
            for h in range(4):
                g128 = float((1.0 - 2.0 ** (-5.0 - h)) ** 128)
                for i in range(NT):
                    gens.append(ret_gen(h, i, it % 2, g128, w_out[h % 2]))
                    it += 1
            prev = None
            for g in gens:
                while True:
                    r, alive = _step(g)
                    if prev is not None:
                        _, pa = _step(prev)
                        if not pa:
                            prev = None
                    if r == 'MID' or not alive:
                        break
                while prev is not None:
                    _, pa = _step(prev)
                    if not pa:
                        prev = None
                prev = g
            while prev is not None:
                _, pa = _step(prev)
                if not pa:
                    prev = None

        for ph in phases:
            AR.off = base_mark
            AR.hi = 0
            if ph == "dsa":
                phase_dsa()
            elif ph == "ffn0":
                phase_ffn(0)
            elif ph == "ret":
                phase_ret()
            elif ph == "ffn1":
                phase_ffn(1)
            P.barrier()
            if DBG.get("verbose"):
                print("phase", ph, "arena hi", AR.hi, {e: len(P.ops[e]) for e in ENGS})
        if store:
            for i in range(NT):
                P.dma("sp", o_v[:, i, :], xs[i][:])
        P.emit(es)
    return nc


def _consts():
    d = 256
    inv = (1.0 / (np.float32(10000.0) ** (np.arange(0, d, 2, dtype=np.float32) / np.float32(d)))).astype(np.float32)
    ang = (np.arange(S, dtype=np.float32)[:, None] * inv[None, :]).astype(np.float32)
    cos = np.cos(ang).astype(np.float32)
    sin = np.sin(ang).astype(np.float32)
    rtab = np.stack([cos, sin], 0).reshape(2, NT, 128, 128).transpose(2, 0, 1, 3)
    hh = np.arange(4, dtype=np.float64)
    gam = 1.0 - 2.0 ** (-5.0 - hh)
    tl = np.arange(128, dtype=np.float64)[:, None] + 1.0
    dq = gam[None, :] ** tl
    dk = gam[None, :] ** (-tl) * (256.0 ** -0.5)
    dtab = np.concatenate([dq, dk], 1).astype(np.float32)
    return np.ascontiguousarray(rtab), np.ascontiguousarray(dtab)


def _prep_shared(inp):
    f = lambda a: np.ascontiguousarray(np.asarray(a, dtype=np.float32))
    cvec1 = np.concatenate([
        f(inp["norm_mix_g"]).reshape(-1), f(inp["norm_ffn_g"]).reshape(-1),
        f(inp["a_q_g"]).reshape(-1), f(inp["a_k_g"]).reshape(-1),
        f(inp["a_iq_g"]).reshape(-1), f(inp["a_ik_g"]).reshape(-1),
        f(inp["b_out_g"]).reshape(-1)])
    assert cvec1.size == NCV
    cvec = np.ascontiguousarray(np.broadcast_to(cvec1[None, :], (128, NCV)))
    cw = f(inp["f_conv_w"])
    cb = f(inp["f_conv_b"])
    conv = np.concatenate([cw, cb[:, None, :]], 1)
    convw = np.ascontiguousarray(conv.reshape(2, 4, 44, 128).transpose(0, 3, 1, 2).reshape(2, 128, 176))
    awi = f(inp["a_w_in"])[0]
    a_w_in = np.ascontiguousarray(np.concatenate(
        [awi[:, 0:1024], awi[:, 1152:1664], awi[:, 1024:1152], awi[:, 1664:1728], awi[:, 1728:1736]], 1))
    bwi = f(inp["b_w_in"])[0]
    q = bwi[:, 0:1024].reshape(D, 4, 256)
    k = bwi[:, 1024:2048].reshape(D, 4, 256)
    v = bwi[:, 2048:4096].reshape(D, 4, 512)
    g = bwi[:, 4096:6144].reshape(D, 4, 512)
    b_w_in = np.ascontiguousarray(np.concatenate([q, k, v, g], 2))
    rtab, dtab = _consts()
    return {
        "cvec": cvec, "convw": convw, "rtab": rtab, "dtab": dtab,
        "a_w_in": a_w_in, "a_w_out": f(inp["a_w_out"])[0],
        "b_w_in": b_w_in, "b_w_out": f(inp["b_w_out"])[0],
        "f_w_up": f(inp["f_w_up"]), "f_w_down": f(inp["f_w_down"]),
    }


LAUNCH_PLAN = [["dsa", "ffn0", "ret", "ffn1"]]
_NC_CACHE = {}


def kernel(**inputs):
    x = np.ascontiguousarray(np.asarray(inputs["x"], dtype=np.float32))
    shared = _prep_shared(inputs)
    n = 8
    cur = [x[c] for c in range(n)]
    for phases in LAUNCH_PLAN:
        key = tuple(phases)
        if key not in _NC_CACHE:
            _NC_CACHE[key] = build(phases)
        nc = _NC_CACHE[key]
        in_maps = []
        need = needed_inputs(phases)
        for c in range(n):
            m = {k: v for k, v in shared.items() if k in need}
            m["x"] = np.ascontiguousarray(cur[c])
            in_maps.append(m)
        res = run_bass_kernel_spmd(nc, in_maps, core_ids=list(range(n)))
        cur = [np.asarray(res.results[c]["out"], dtype=np.float32) for c in range(n)]
    return np.stack(cur, 0).astype(np.float32)
```
